# Optimizing a Trainium2 kernel written in Bass

```python
import jax
import jax.numpy as jnp
from jax import lax
import numpy as np

D_MODEL = 1024
BATCH = 8
SEQ = 2048
DEPTH = 2

CHUNK = 64
MEM_LEN = 256
RET_HEADS = 4
RET_HD = 128
RET_W = RET_HEADS * RET_HD
ROPE_BASE = 10000.0
RWKV_HEADS = 8
RWKV_HD = 64
RWKV_W = RWKV_HEADS * RWKV_HD
W_LORA = 64
A_LORA = 64
G_LORA = 128
RWKV_GN_EPS = 64e-5
XA_HEADS = 4
XA_HD = D_MODEL // XA_HEADS
N_EXPERTS = 16
N_GROUPS = 4
EXPERTS_PER_GROUP = N_EXPERTS // N_GROUPS
TOP_K = 2
D_EXPERT = 512
ALPHA = (2 * DEPTH) ** 0.25
BETA = (8 * DEPTH) ** -0.25
LN_EPS = 1e-5
RET_COLS = 4 * RET_W
RWKV_COLS = 3 * RWKV_W + W_LORA + A_LORA + G_LORA
GATE_COLS = 2 * D_MODEL
N_IN = RET_COLS + RWKV_COLS + GATE_COLS

kernel_name = 'hybrid_retention_rwkv7_memxattn_groupmoe_deepnorm'


def layer_norm(x, g, b, eps=LN_EPS):
    xf = x.astype(jnp.float32)
    mu = jnp.mean(xf, axis=-1, keepdims=True)
    var = jnp.mean(jnp.square(xf - mu), axis=-1, keepdims=True)
    y = (xf - mu) * lax.rsqrt(var + eps)
    return (y * g.astype(jnp.float32) + b.astype(jnp.float32)).astype(x.dtype)


def group_norm(y, eps):
    yf = y.astype(jnp.float32)
    mu = jnp.mean(yf, axis=-1, keepdims=True)
    var = jnp.mean(jnp.square(yf - mu), axis=-1, keepdims=True)
    yn = (yf - mu) * lax.rsqrt(var + eps)
    return yn.reshape(*y.shape[:-2], -1).astype(y.dtype)


def rotary(t, pos):
    half = t.shape[-1] // 2
    inv_freq = ROPE_BASE ** (-jnp.arange(half, dtype=jnp.float32) / half)
    ang = pos.astype(jnp.float32)[:, None] * inv_freq[None, :]
    cos = jnp.cos(ang)[None, :, None, :].astype(t.dtype)
    sin = jnp.sin(ang)[None, :, None, :].astype(t.dtype)
    t1, t2 = t[..., :half], t[..., half:]
    return jnp.concatenate([t1 * cos - t2 * sin, t1 * sin + t2 * cos], axis=-1)


def retention(q, k, v):
    B, S, H, hd = q.shape
    N = S // CHUNK
    dt = q.dtype
    log_gamma = jnp.log(1.0 - 2.0 ** (-5.0 - jnp.arange(H, dtype=jnp.float32)))
    c = jnp.arange(CHUNK, dtype=jnp.float32)
    inner_mask = jnp.exp(log_gamma[:, None, None] * jnp.abs(c[:, None] - c[None, :])).astype(dt)
    k_decay = jnp.exp(log_gamma[:, None] * (CHUNK - 1 - c)).astype(dt)
    q_decay = jnp.exp(log_gamma[:, None] * (c + 1.0)).astype(dt)
    n = jnp.arange(N, dtype=jnp.float32)
    dn = n[:, None] - 1.0 - n[None, :]
    chunk_decay = jnp.where(dn >= 0, jnp.exp(log_gamma[:, None, None] * CHUNK * jnp.maximum(dn, 0.0)), 0.0).astype(dt)
    to_chunks = lambda t: t.reshape(B, N, CHUNK, H, hd).transpose(0, 3, 1, 2, 4)
    qc, kc, vc = to_chunks(q), to_chunks(k * hd ** -0.5), to_chunks(v)
    scores = jnp.einsum('bhncd,bhnmd->bhncm', qc, kc) * inner_mask[:, None]
    inner = jnp.einsum('bhncm,bhnme->bhnce', scores, vc)
    kv = jnp.einsum('bhncd,bhnce->bhnde', kc * k_decay[:, None, :, None], vc)
    state_in = jnp.einsum('hni,bhide->bhnde', chunk_decay, kv)
    cross = jnp.einsum('bhncd,bhnde->bhnce', qc * q_decay[:, None, :, None], state_in)
    y = inner + cross
    return y.transpose(0, 2, 3, 1, 4).reshape(B, S, H, hd)


def token_shift(p, mu):
    prev = jnp.pad(p[:, :-1], ((0, 0), (1, 0), (0, 0)))
    return p + mu * (prev - p)


def rwkv7_time_mix(p, w_up, w0, a_up, a0, g_up, k_k, k_a, r_k, ln_g, ln_b):
    B, S, _ = p.shape
    dt = p.dtype
    r, k, v, dw, da, dg = jnp.split(
        p, [RWKV_W, 2 * RWKV_W, 3 * RWKV_W, 3 * RWKV_W + W_LORA, 3 * RWKV_W + W_LORA + A_LORA], axis=-1)
    w_log = -jax.nn.softplus(-(w0 + jnp.tanh(dw) @ w_up)) - 0.5
    decay = jnp.exp(-jnp.exp(w_log.astype(jnp.float32))).astype(dt)
    a = jax.nn.sigmoid(a0 + da @ a_up)
    g = jax.nn.sigmoid(dg) @ g_up
    heads = lambda t: t.reshape(B, S, RWKV_HEADS, RWKV_HD)
    kk = heads(k * k_k).astype(jnp.float32)
    kk = (kk / jnp.maximum(jnp.linalg.norm(kk, axis=-1, keepdims=True), 1e-12)).astype(dt)
    k = k * (1.0 + (a - 1.0) * k_a)
    r_h, k_h, v_h, a_h, w_h = heads(r), heads(k), heads(v), heads(a), heads(decay)

    def step(state, inp):
        r_t, w_t, k_t, v_t, kk_t, a_t = inp
        sa = jnp.einsum('bhvk,bhk->bhv', state, -kk_t)
        state = (state * w_t[:, :, None, :]
                 + sa[..., None] * (kk_t * a_t)[:, :, None, :]
                 + v_t[..., None] * k_t[:, :, None, :])
        return state, jnp.einsum('bhvk,bhk->bhv', state, r_t)

    time_major = lambda t: jnp.swapaxes(t, 0, 1).astype(dt)
    s0 = jnp.zeros((B, RWKV_HEADS, RWKV_HD, RWKV_HD), dt)
    _, y = lax.scan(step, s0, (time_major(r_h), time_major(w_h), time_major(k_h),
                               time_major(v_h), time_major(kk), time_major(a_h)))
    y = jnp.swapaxes(y, 0, 1)
    y = group_norm(y, RWKV_GN_EPS) * ln_g + ln_b
    bonus = jnp.sum(r_h * k_h * r_k, axis=-1, keepdims=True) * v_h
    return (y + bonus.reshape(B, S, RWKV_W)) * g


def hybrid_mixer(x, w_in, ret_gn_g, rwkv_mu, rwkv_w_up, rwkv_w0, rwkv_a_up, rwkv_a0, rwkv_g_up,
                 rwkv_k_k, rwkv_k_a, rwkv_r_k, rwkv_ln_g, rwkv_ln_b, w_ret_up, w_rwkv_up, w_out):
    B, S, _ = x.shape
    p = x @ w_in
    p_ret = p[..., :RET_COLS]
    p_rwkv = p[..., RET_COLS:RET_COLS + RWKV_COLS]
    p_gate = p[..., RET_COLS + RWKV_COLS:]
    q, k, v, g = jnp.split(p_ret, 4, axis=-1)
    heads = lambda t: t.reshape(B, S, RET_HEADS, RET_HD)
    pos = jnp.arange(S)
    y_ret = retention(rotary(heads(q), pos), rotary(heads(k), pos), heads(v))
    y_ret = jax.nn.silu(g) * (group_norm(y_ret, LN_EPS) * ret_gn_g)
    y_rwkv = rwkv7_time_mix(token_shift(p_rwkv, rwkv_mu), rwkv_w_up, rwkv_w0, rwkv_a_up, rwkv_a0,
                            rwkv_g_up, rwkv_k_k, rwkv_k_a, rwkv_r_k, rwkv_ln_g, rwkv_ln_b)
    gate_ret, gate_rwkv = jnp.split(jax.nn.sigmoid(p_gate), 2, axis=-1)
    merged = gate_ret * (y_ret @ w_ret_up) + gate_rwkv * (y_rwkv @ w_rwkv_up)
    return merged @ w_out


def memory_cross_attention(x, mem, wq, wkv, wo):
    B, S, _ = x.shape
    M = mem.shape[1]
    q = (x @ wq).reshape(B, S, XA_HEADS, XA_HD)
    k, v = jnp.split(mem @ wkv, 2, axis=-1)
    k = k.reshape(B, M, XA_HEADS, XA_HD)
    v = v.reshape(B, M, XA_HEADS, XA_HD)
    s = jnp.einsum('bshd,bmhd->bhsm', q, k).astype(jnp.float32) * XA_HD ** -0.5
    probs = jax.nn.softmax(s, axis=-1).astype(x.dtype)
    o = jnp.einsum('bhsm,bmhd->bshd', probs, v).reshape(B, S, D_MODEL)
    return o @ wo


def grouped_moe(x, router_w, router_bias, w_gate, w_up, w_down):
    B, S, D = x.shape
    t = x.reshape(-1, D)
    affinity = jax.nn.sigmoid((t @ router_w).astype(jnp.float32))
    choice = affinity + router_bias.astype(jnp.float32)
    group_score = jnp.sum(lax.top_k(choice.reshape(-1, N_GROUPS, EXPERTS_PER_GROUP), TOP_K)[0], axis=-1)
    best_group = jnp.argmax(group_score, axis=-1)
    in_group = (jnp.arange(N_EXPERTS) // EXPERTS_PER_GROUP)[None, :] == best_group[:, None]
    _, idx = lax.top_k(jnp.where(in_group, choice, -jnp.inf), TOP_K)
    wts = jnp.take_along_axis(affinity, idx, axis=-1)
    wts = wts / jnp.sum(wts, axis=-1, keepdims=True)
    combine = jnp.sum(jax.nn.one_hot(idx, N_EXPERTS, dtype=jnp.float32) * wts[..., None], axis=1).astype(x.dtype)
    y = jnp.zeros_like(t)
    for e in range(N_EXPERTS):
        h = jax.nn.silu(t @ w_gate[e]) * (t @ w_up[e])
        y = y + combine[:, e:e + 1] * (h @ w_down[e])
    return y.reshape(B, S, D)


def setup_inputs(seed: int = 0) -> dict:
    key = jax.random.key(seed)
    ks = iter(jax.random.split(key, 48))
    nrm = lambda shape, scale: scale * jax.random.normal(next(ks), shape, jnp.float32)
    L = DEPTH
    return {
        'x': nrm((BATCH, SEQ, D_MODEL), 1.0),
        'mem': nrm((BATCH, MEM_LEN, D_MODEL), 1.0),
        'ln_in_g': 1.0 + nrm((D_MODEL,), 0.02),
        'ln_in_b': nrm((D_MODEL,), 0.02),
        'router_w': nrm((D_MODEL, N_EXPERTS), D_MODEL ** -0.5),
        'router_bias': nrm((N_EXPERTS,), 0.01),
        'w_in': nrm((L, D_MODEL, N_IN), D_MODEL ** -0.5),
        'ret_gn_g': 1.0 + nrm((L, RET_W), 0.02),
        'rwkv_mu': jax.random.uniform(next(ks), (L, RWKV_COLS), jnp.float32),
        'rwkv_w_up': nrm((L, W_LORA, RWKV_W), 0.5 * W_LORA ** -0.5),
        'rwkv_w0': jnp.linspace(-6.0, -1.0, RWKV_W, dtype=jnp.float32)[None, :] + nrm((L, RWKV_W), 0.1),
        'rwkv_a_up': nrm((L, A_LORA, RWKV_W), 0.5 * A_LORA ** -0.5),
        'rwkv_a0': nrm((L, RWKV_W), 0.1),
        'rwkv_g_up': nrm((L, G_LORA, RWKV_W), G_LORA ** -0.5),
        'rwkv_k_k': 0.85 + nrm((L, RWKV_W), 0.05),
        'rwkv_k_a': 1.0 + nrm((L, RWKV_W), 0.05),
        'rwkv_r_k': nrm((L, RWKV_HEADS, RWKV_HD), 0.1),
        'rwkv_ln_g': 1.0 + nrm((L, RWKV_W), 0.02),
        'rwkv_ln_b': nrm((L, RWKV_W), 0.02),
        'w_ret_up': nrm((L, RET_W, D_MODEL), RET_W ** -0.5),
        'w_rwkv_up': nrm((L, RWKV_W, D_MODEL), RWKV_W ** -0.5),
        'w_out': nrm((L, D_MODEL, D_MODEL), BETA * D_MODEL ** -0.5),
        'ln1_g': 1.0 + nrm((L, D_MODEL), 0.02),
        'ln1_b': nrm((L, D_MODEL), 0.02),
        'xa_wq': nrm((L, D_MODEL, D_MODEL), D_MODEL ** -0.5),
        'xa_wkv': nrm((L, D_MODEL, 2 * D_MODEL), D_MODEL ** -0.5),
        'xa_wo': nrm((L, D_MODEL, D_MODEL), BETA * D_MODEL ** -0.5),
        'ln2_g': 1.0 + nrm((L, D_MODEL), 0.02),
        'ln2_b': nrm((L, D_MODEL), 0.02),
        'moe_w_gate': nrm((L, N_EXPERTS, D_MODEL, D_EXPERT), D_MODEL ** -0.5),
        'moe_w_up': nrm((L, N_EXPERTS, D_MODEL, D_EXPERT), D_MODEL ** -0.5),
        'moe_w_down': nrm((L, N_EXPERTS, D_EXPERT, D_MODEL), BETA * D_EXPERT ** -0.5),
        'ln3_g': 1.0 + nrm((L, D_MODEL), 0.02),
        'ln3_b': nrm((L, D_MODEL), 0.02),
    }


def reference(x, mem, ln_in_g, ln_in_b, router_w, router_bias, w_in, ret_gn_g, rwkv_mu, rwkv_w_up,
              rwkv_w0, rwkv_a_up, rwkv_a0, rwkv_g_up, rwkv_k_k, rwkv_k_a, rwkv_r_k, rwkv_ln_g, rwkv_ln_b,
              w_ret_up, w_rwkv_up, w_out, ln1_g, ln1_b, xa_wq, xa_wkv, xa_wo, ln2_g, ln2_b,
              moe_w_gate, moe_w_up, moe_w_down, ln3_g, ln3_b):
    x = layer_norm(x, ln_in_g, ln_in_b)
    for l in range(DEPTH):
        h = hybrid_mixer(x, w_in[l], ret_gn_g[l], rwkv_mu[l], rwkv_w_up[l], rwkv_w0[l], rwkv_a_up[l],
                         rwkv_a0[l], rwkv_g_up[l], rwkv_k_k[l], rwkv_k_a[l], rwkv_r_k[l], rwkv_ln_g[l],
                         rwkv_ln_b[l], w_ret_up[l], w_rwkv_up[l], w_out[l])
        x = layer_norm(ALPHA * x + h, ln1_g[l], ln1_b[l])
        h = memory_cross_attention(x, mem, xa_wq[l], xa_wkv[l], xa_wo[l])
        x = layer_norm(ALPHA * x + h, ln2_g[l], ln2_b[l])
        h = grouped_moe(x, router_w, router_bias, moe_w_gate[l], moe_w_up[l], moe_w_down[l])
        x = layer_norm(ALPHA * x + h, ln3_g[l], ln3_b[l])
    return x
```

```python
import numpy as np
import ml_dtypes
from contextlib import ExitStack
import concourse.bass as bass
import concourse.mybir as mybir
from concourse.bass_utils import run_bass_kernel_spmd

F32 = mybir.dt.float32
BF16 = mybir.dt.bfloat16
AF = mybir.ActivationFunctionType
ALU = mybir.AluOpType
AX = mybir.AxisListType

D = 1024
S = 2048
NT = 16
DEPTH = 2
ALPHA = float((2 * DEPTH) ** 0.25)
LN_EPS = 1e-5
N_IN = 5888
NDS = 8
EPOCH = 16000
C0 = float(np.exp(-0.5))


def _prod(xs):
    r = 1
    for v in xs:
        r *= int(v)
    return r


def _dsize(dt):
    return int(mybir.dt.size(dt))


class Prog:
    ENGS = ("pe", "dve", "act", "pool", "sp")

    def __init__(self, nc, es, arena_bytes):
        self.nc = nc
        self.es = es
        self.eng = {"pe": nc.tensor, "dve": nc.vector, "act": nc.scalar,
                    "pool": nc.gpsimd, "sp": nc.sync}
        self.q = {k: [] for k in self.ENGS}
        self.esem = {k: [es.enter_context(nc.semaphore(f"e_{k}_{i}")) for i in range(3)]
                     for k in self.ENGS}
        self.dsem = {k: [es.enter_context(nc.semaphore(f"d_{k}_{i}")) for i in range(NDS)]
                     for k in ("sp", "pool", "act")}
        self.cnt = {k: 0 for k in self.ENGS}
        self.dcnt = {k: [0] * NDS for k in self.dsem}
        self.dnext = {k: 0 for k in self.dsem}
        self.waited = {k: {} for k in self.ENGS}
        self.acc = {}
        self.base = {}
        self.dram = set()
        self.n_instr = 0
        self.out_tokens = []
        slab = es.enter_context(nc.sbuf_tensor("slab", [128, arena_bytes // 4], F32))
        self.arena_lo = int(nc.lookup_mloc("slab").addr)
        self.arena_hi = self.arena_lo + arena_bytes
        self.ptr = self.arena_lo
        self.top = self.arena_hi
        self.uid = 0
        self.x_live = True
        self.bank_ptr = 0
        import os
        self.max_instr = int(os.environ.get("MAXI", str(10 ** 9)))
        self.ps = es.enter_context(nc.psum_tensor("ps", [128, 4096], F32))
        self.base["ps"] = ("PS", 0)
        self.psb = self.ps.bitcast(BF16)

    def alloc(self, name, shape, dtype, top=False):
        nbytes = _prod(shape[1:]) * _dsize(dtype)
        nbytes = (nbytes + 31) // 32 * 32
        if top:
            self.top -= nbytes
            off = self.top
        else:
            off = self.ptr
            self.ptr += nbytes
        lim = self.top if self.x_live else self.arena_hi
        assert self.ptr <= lim, f"SBUF arena overflow at {name}: {self.ptr} > {lim}"
        self.uid += 1
        t = self.nc.alloc_sbuf_tensor_at(f"{name}_{self.uid}", list(shape), dtype, offset=off)
        self.base[t.name] = ("SB", off)
        return t

    def alloc_at(self, name, shape, dtype, off):
        self.uid += 1
        t = self.nc.alloc_sbuf_tensor_at(f"{name}_{self.uid}", list(shape), dtype, offset=off)
        self.base[t.name] = ("SB", off)
        return t

    def mark(self):
        return (self.ptr, self.top)

    def release(self, m):
        self.ptr, self.top = m

    def dram_tensor(self, name, shape, dtype, kind):
        t = self.nc.dram_tensor(name, list(shape), dtype, kind=kind)
        self.dram.add(name)
        return t.ap()

    bank_rng = (0, 8)

    def rr(self, n=1):
        lo, hi = self.bank_rng
        if not hasattr(self, "bank_ptrs"):
            self.bank_ptrs = {}
        p = self.bank_ptrs.get((lo, hi), lo)
        b = (p + n - 1) // n * n
        if b + n > hi:
            b = lo
        self.bank_ptrs[(lo, hi)] = b + n
        return b

    def bank(self, b, n=1):
        return self.ps[:, b * 512:(b + n) * 512]

    def bankb(self, b, n=1):
        return self.psb[:, b * 1024:(b + n) * 1024]

    def _region(self, a):
        t = a.tensor
        name = t.name
        dims = list(a.ap)
        off = int(a.offset)
        sz = _dsize(a.dtype)
        if name in self.dram:
            ext = sum((int(c) - 1) * abs(int(s)) for s, c in dims)
            return ("D:" + name, 0, 1, off * sz, (off + ext + 1) * sz)
        space, base = self.base[name]
        F = _prod(list(t.shape)[1:])
        p0 = off // F
        f0 = off % F
        npart = int(dims[0][1])
        ext = sum((int(c) - 1) * abs(int(s)) for s, c in dims[1:])
        lo = base + f0 * sz
        hi = base + (f0 + ext + 1) * sz
        p1 = p0 + npart
        if space == "PS":
            lo = lo // 2048 * 2048
            hi = (hi + 2047) // 2048 * 2048
            p0 = p0 // 32 * 32
            p1 = (p1 + 31) // 32 * 32
        return (space, p0, p1, lo, hi)

    @staticmethod
    def _overlap(r1, r2):
        return r1[1] < r2[2] and r2[1] < r1[2] and r1[3] < r2[4] and r2[3] < r1[4]

    @staticmethod
    def _covers(r1, r2):
        return r1[1] <= r2[1] and r1[2] >= r2[2] and r1[3] <= r2[3] and r1[4] >= r2[4]

    def _deps(self, reads, writes):
        toks = {}
        rregs = [self._region(a) for a in reads]
        wregs = [self._region(a) for a in writes]
        for r in rregs:
            ps = r[0] == "PS"
            for (reg, key, val, isw) in self.acc.get(r[0], ()):
                if (isw or ps) and self._overlap(reg, r) and toks.get(key, 0) < val:
                    toks[key] = val
        for r in wregs:
            for (reg, key, val, isw) in self.acc.get(r[0], ()):
                if self._overlap(reg, r) and toks.get(key, 0) < val:
                    toks[key] = val
        return toks, rregs, wregs

    def _record(self, rregs, wregs, key, val):
        for r in wregs:
            lst = self.acc.setdefault(r[0], [])
            lst[:] = [e for e in lst if not self._covers(r, e[0])]
            lst.append((r, key, val, True))
        for r in rregs:
            lst = self.acc.setdefault(r[0], [])
            lst[:] = [e for e in lst
                      if not ((not e[3]) and e[1] == key and self._covers(r, e[0]))]
            lst.append((r, key, val, False))

    def _waits_for(self, e, toks):
        ws = []
        for key, val in toks.items():
            if key == ("e", e) and e == "pe":
                continue
            if self.waited[e].get(key, 0) < val:
                self.waited[e][key] = val
                ws.append((key, val))
        return ws

    max_instr = 10 ** 9
    _cap = None
    import os as _os
    zip_gran = int(_os.environ.get("ZGALL", "1"))

    def capture(self, f):
        saved = self._cap
        self._cap = []
        f()
        lst = self._cap
        self._cap = saved
        return lst

    def replay(self, lists):
        idx = [0] * len(lists)
        tot = [max(len(l), 1) for l in lists]
        while True:
            best, bf = -1, 2.0
            for i, l in enumerate(lists):
                if idx[i] < len(l):
                    fr = idx[i] / tot[i]
                    if fr < bf:
                        best, bf = i, fr
            if best < 0:
                break
            for _ in range(self.zip_gran):
                if idx[best] >= len(lists[best]):
                    break
                it = lists[best][idx[best]]
                idx[best] += 1
                if it[0] == "op":
                    self.op(it[1], it[2], it[3], it[4])
                else:
                    self.dma(it[1], it[2], it[3], it[4], **it[5])

    def zipped(self, fns, split=None):
        if self._cap is not None or len(fns) == 1:
            for f in fns:
                f()
            return
        lo0, hi0 = self.bank_rng
        if split is None:
            split = [(hi0 - lo0) // len(fns)] * len(fns)
        lists = []
        lo = lo0
        for k, f in enumerate(fns):
            self.bank_rng = (lo, lo + split[k])
            lo += split[k]
            lists.append(self.capture(f))
        self.bank_rng = (lo0, hi0)
        self.replay(lists)

    def op(self, e, fn, reads=(), writes=()):
        if self._cap is not None:
            self._cap.append(("op", e, fn, tuple(reads), tuple(writes)))
            return
        if self.n_instr >= self.max_instr:
            return
        toks, rregs, wregs = self._deps(reads, writes)
        ws = self._waits_for(e, toks)
        if self.n_instr + 1 == self.max_instr:
            print("LAST INSTR", e, "reads", [(a.tensor.name, a.offset, a.ap) for a in reads], "writes", [(a.tensor.name, a.offset, a.ap) for a in writes], "waits", ws, "cnt", self.cnt)
        self.cnt[e] += 1
        key = ("e", e)
        val = self.cnt[e]
        self.q[e].append((ws, fn, key, val))
        self._record(rregs, wregs, key, val)
        self.n_instr += 1

    def dma(self, qn, out, in_, is_output=False, **kw):
        if self._cap is not None:
            self._cap.append(("dma", qn, out, in_, is_output, kw))
            return
        if self.n_instr >= self.max_instr and not is_output:
            return
        toks, rregs, wregs = self._deps([in_], [out])
        i = self.dnext[qn]
        self.dnext[qn] = (i + 1) % NDS
        key = ("d", qn, i)
        if self.dcnt[qn][i] > 0:
            toks[key] = max(toks.get(key, 0), self.dcnt[qn][i])
        ws = self._waits_for(qn, toks)
        self.dcnt[qn][i] += 16
        val = self.dcnt[qn][i]

        def fn(eng, out=out, in_=in_, kw=kw):
            return eng.dma_start(out=out, in_=in_, **kw)
        self.q[qn].append((ws, fn, key, val))
        self._record(rregs, wregs, key, val)
        self.n_instr += 1
        if is_output:
            self.out_tokens.append((key, val))

    def _sem(self, key, val):
        if key[0] == "e":
            ep = (val - 1) // EPOCH
            return self.esem[key[1]][ep], (val - 1) % EPOCH + 1
        return self.dsem[key[1]][key[2]], val

    def emit(self):
        block = self.es.enter_context(self.nc.Block())
        final = {}
        for key, val in self.out_tokens:
            final[key] = max(final.get(key, 0), val)
        prog = self

        def make(e):
            def body(eng):
                for (ws, fn, key, val) in prog.q[e]:
                    for (k, v) in ws:
                        s, sv = prog._sem(k, v)
                        eng.wait_ge(s, sv)
                    ins = fn(eng)
                    s, sv = prog._sem(key, val)
                    ins.then_inc(s, 16 if key[0] == "d" else 1)
                if e == "sp":
                    for k, v in final.items():
                        s, sv = prog._sem(k, v)
                        eng.wait_ge(s, sv)
            return body
        block.tensor(make("pe"))
        block.vector(make("dve"))
        block.scalar(make("act"))
        block.gpsimd(make("pool"))
        block.sync(make("sp"))

    def mm(self, out, lhsT, rhs, start=True, stop=True):
        self.op("pe", lambda e: e.matmul(out, lhsT=lhsT, rhs=rhs, start=start, stop=stop),
                reads=[lhsT, rhs], writes=[out])

    def tr(self, out, in_, ident):
        self.op("pe", lambda e: e.transpose(out, in_, ident), reads=[in_, ident], writes=[out])

    def act(self, out, in_, func, bias=None, scale=1.0, accum_out=None, eng="act"):
        reads = [in_]
        kw = {}
        if bias is not None:
            kw["bias"] = bias
            if not isinstance(bias, (int, float)):
                reads.append(bias)
        if not isinstance(scale, (int, float)):
            reads.append(scale)
        writes = [out]
        if accum_out is not None:
            kw["accum_out"] = accum_out
            writes.append(accum_out)
        self.op("act", lambda e: e.activation(out=out, in_=in_, func=func, scale=scale, **kw),
                reads=reads, writes=writes)

    def ts(self, out, in0, s1, op0, s2=None, op1=None, eng="dve", accum_out=None):
        reads = [in0]
        for s in (s1, s2):
            if s is not None and not isinstance(s, (int, float)):
                reads.append(s)
        kw = {}
        writes = [out]
        if accum_out is not None:
            kw["accum_out"] = accum_out
            writes.append(accum_out)
        if op1 is None:
            self.op(eng, lambda e: e.tensor_scalar(out=out, in0=in0, scalar1=s1, scalar2=None, op0=op0, **kw),
                    reads=reads, writes=writes)
        else:
            self.op(eng, lambda e: e.tensor_scalar(out=out, in0=in0, scalar1=s1, scalar2=s2,
                                                   op0=op0, op1=op1, **kw),
                    reads=reads, writes=writes)

    def tt(self, out, in0, in1, op, eng="dve"):
        self.op(eng, lambda e: e.tensor_tensor(out=out, in0=in0, in1=in1, op=op),
                reads=[in0, in1], writes=[out])

    def stt(self, out, in0, scalar, in1, op0, op1):
        reads = [in0, in1]
        if not isinstance(scalar, (int, float)):
            reads.append(scalar)
        self.op("dve", lambda e: e.scalar_tensor_tensor(out=out, in0=in0, scalar=scalar, in1=in1,
                                                        op0=op0, op1=op1),
                reads=reads, writes=[out])

    def reduce(self, out, in_, op, axis=AX.X):
        self.op("dve", lambda e: e.tensor_reduce(out=out, in_=in_, axis=axis, op=op), reads=[in_], writes=[out])

    def recip(self, out, in_):
        self.op("dve", lambda e: e.reciprocal(out=out, in_=in_), reads=[in_], writes=[out])

    def copy(self, out, in_, eng="dve"):
        if eng == "act":
            self.act(out, in_, AF.Copy)
        else:
            self.op(eng, lambda e: e.tensor_copy(out=out, in_=in_), reads=[in_], writes=[out])

    def memset(self, out, val, eng="dve"):
        self.op(eng, lambda e: e.memset(out, val), reads=[], writes=[out])


def _consts():
    c = {}
    bf = ml_dtypes.bfloat16
    c["ident"] = np.eye(128, dtype=np.float32).astype(bf)
    blk = np.zeros((128, 128), np.float32)
    blk[:64, :64] = 1.0
    blk[64:, 64:] = 1.0
    c["blkones"] = blk.astype(bf)
    sw = np.zeros((128, 128), np.float32)
    for d in range(128):
        sw[(d + 64) % 128, d] = 1.0
    c["pswap"] = sw.astype(bf)
    half = 64
    inv = (10000.0 ** (-np.arange(half, dtype=np.float32) / half)).astype(np.float32)
    ang = (np.arange(S, dtype=np.float32)[:, None] * inv[None, :]).astype(np.float32)
    cs = np.cos(ang.astype(np.float64)).T
    sn = np.sin(ang.astype(np.float64)).T
    c["cos"] = np.concatenate([cs, cs], 0).astype(np.float32).astype(bf)
    c["sins"] = np.concatenate([-sn, sn], 0).astype(np.float32).astype(bf)
    lg = np.log(1.0 - 2.0 ** (-5.0 - np.arange(4, dtype=np.float64)))
    m = np.arange(128)[:, None]
    cc = np.arange(128)[None, :]
    same = (m // 64) == (cc // 64)
    later = (m // 64) < (cc // 64)
    rmask = np.zeros((4, 128, 128))
    for h in range(4):
        rmask[h] = np.where(same, np.exp(lg[h] * np.abs(cc - m)), 0.0) + np.where(later, np.exp(lg[h] * (cc - m)), 0.0)
    rmask *= 128.0 ** -0.5
    c["rmask"] = np.ascontiguousarray(rmask.transpose(1, 0, 2)).astype(np.float32)
    qdec = np.exp(lg[:, None] * (np.arange(128)[None, :] + 1.0))
    c["qdec"] = np.ascontiguousarray(np.broadcast_to(np.tile(qdec, (1, 4))[None], (128, 4, 512))).astype(np.float32).astype(bf)
    kdec = np.exp(lg[:, None] * (127.0 - np.arange(128)[None, :])) * 128.0 ** -0.5
    c["kdec"] = np.ascontiguousarray(kdec.T).astype(np.float32)
    c["g128"] = np.exp(lg * 128.0)
    s_ = np.arange(64)[:, None]
    t_ = np.arange(64)[None, :]
    strict = (t_ > s_).astype(np.float32)
    incl = (t_ >= s_).astype(np.float32)
    m1 = np.concatenate([strict, incl], 1)
    c["wmask1"] = np.ascontiguousarray(np.broadcast_to(m1[:, None, :], (64, 16, 128))).astype(np.float32).astype(bf)
    low = (t_ < s_).astype(np.float32)
    c["wmask2"] = np.ascontiguousarray(np.broadcast_to(low[:, None, :], (64, 8, 64))).astype(np.float32).astype(bf)
    c["identrep"] = np.ascontiguousarray(np.broadcast_to(np.eye(64, dtype=np.float32)[:, None, :], (64, 8, 64))).astype(bf)
    rst = np.ones((128, 4, 256), np.float32)
    rst[:, :, ::64] = 0.0
    c["rstmask"] = rst
    return c


CONSTS = None


def _get_consts():
    global CONSTS
    if CONSTS is None:
        CONSTS = _consts()
    return CONSTS


_MYDT = {np.dtype(np.float32): F32, np.dtype(ml_dtypes.bfloat16): BF16}

PARAM_SHAPES = {
    'ln_in_g': (1024,), 'ln_in_b': (1024,), 'router_w': (1024, 16), 'router_bias': (16,),
    'w_in': (2, 1024, 5888), 'ret_gn_g': (2, 512), 'rwkv_mu': (2, 1792), 'rwkv_w_up': (2, 64, 512),
    'rwkv_w0': (2, 512), 'rwkv_a_up': (2, 64, 512), 'rwkv_a0': (2, 512), 'rwkv_g_up': (2, 128, 512),
    'rwkv_k_k': (2, 512), 'rwkv_k_a': (2, 512), 'rwkv_r_k': (2, 8, 64), 'rwkv_ln_g': (2, 512),
    'rwkv_ln_b': (2, 512), 'w_ret_up': (2, 512, 1024), 'w_rwkv_up': (2, 512, 1024),
    'w_out': (2, 1024, 1024), 'ln1_g': (2, 1024), 'ln1_b': (2, 1024), 'xa_wq': (2, 1024, 1024),
    'xa_wkv': (2, 1024, 2048), 'xa_wo': (2, 1024, 1024), 'ln2_g': (2, 1024), 'ln2_b': (2, 1024),
    'moe_w_gate': (2, 16, 1024, 512), 'moe_w_up': (2, 16, 1024, 512), 'moe_w_down': (2, 16, 512, 1024),
    'ln3_g': (2, 1024), 'ln3_b': (2, 1024),
}


class K:
    def __init__(self, n_layers=DEPTH, stage="full", taps=()):
        self.n_layers = n_layers
        self.stage = stage
        self.taps = set(taps)
        self.tap_out = {}

    def tap(self, name, ap, shape, dtype):
        if name not in self.taps:
            return
        P = self.P
        d = P.dram_tensor("tap_" + name, shape, dtype, "ExternalOutput")
        P.dma("sp", d, ap, is_output=True)
        self.tap_out[name] = "tap_" + name

    def build(self):
        nc = bass.Bass("TRN2", target_bir_lowering=False)
        self.nc = nc
        self.es = ExitStack()
        es = self.es
        import os
        P = Prog(nc, es, int(os.environ.get('ARENA_KB', '207')) * 1024)
        self.P = P
        self.x_d = P.dram_tensor("x", [S, D], F32, "ExternalInput")
        self._mem_d = None
        self._out_d = None
        self._xs_d = None
        self.prm = {}
        self.lazy = self.stage != "full"
        for k, shp in PARAM_SHAPES.items():
            pass
        self.cst_d = {}
        self.XT = P.alloc("XT", [128, 8, S], BF16)
        self.ident = P.alloc("ident", [128, 128], BF16)
        P.dma("sp", self.ident[:], self.cst("ident"))
        self.blkones = P.alloc("blkones", [128, 128], BF16)
        P.dma("sp", self.blkones[:], self.cst("blkones"))
        self.eps_t = P.alloc("eps", [128, 1], F32)
        P.memset(self.eps_t[:], LN_EPS)
        self.lnG, self.lnB, self.lnGT, self.lnBT, self.xnb = [None], [None], [None], [None], None
        self.X = P.alloc("X", [128, NT, D], F32, top=True)
        self.lnst = [P.alloc(f"lnst{i}", [128, 2, 6], F32) for i in range(4)]
        self.lnmv = [P.alloc(f"lnmv{i}", [128, 2], F32) for i in range(4)]
        self.lnr = [P.alloc(f"lnr{i}", [128, 2], F32) for i in range(4)]
        self.tile_ctr = 0

        self.phase_input()
        if self.stage not in ("ln_in", "ret", "rwkv", "merge", "x1"):
            self.phase_mem()
        if self.stage == "ln_in":
            self.store_X()
            return self.finish()
        for l in range(self.n_layers):
            self.layer(l)
            if self.stage != "full":
                return self.finish()
        self.store_X()
        return self.finish()

    def cst(self, k):
        if k not in self.cst_d:
            v = _get_consts()[k]
            self.cst_d[k] = self.P.dram_tensor("c_" + k, list(v.shape), _MYDT[v.dtype], "ExternalInput")
        return self.cst_d[k]

    @property
    def mem_d(self):
        if self._mem_d is None:
            self._mem_d = self.P.dram_tensor("mem", [256, D], F32, "ExternalInput")
        return self._mem_d

    @property
    def out_d(self):
        if self._out_d is None:
            self._out_d = self.P.dram_tensor("out", [S, D], F32, "ExternalOutput")
        return self._out_d

    @property
    def xs_d(self):
        if self._xs_d is None:
            self._xs_d = self.P.dram_tensor("xspill", [S, D], F32, "Internal")
        return self._xs_d

    def prm_(self, k):
        if k not in self.prm:
            self.prm[k] = self.P.dram_tensor(k, list(PARAM_SHAPES[k]), F32, "ExternalInput")
        return self.prm[k]

    def finish(self):
        self.P.emit()
        return self.nc

    def store_X(self):
        P = self.P
        for i in range(NT):
            P.dma("sp", self.out_d[i * 128:(i + 1) * 128, :], self.X[:, i, :], is_output=True)

    def load_ln(self, g_ap, b_ap):
        P = self.P
        i = 0
        self.lnG[0] = P.alloc("lnG", [128, D], F32)
        self.lnB[0] = P.alloc("lnB", [128, D], F32)
        self.lnGT[0] = P.alloc("lnGT", [128, 8], F32)
        self.lnBT[0] = P.alloc("lnBT", [128, 8], F32)
        self.xnb = [P.alloc(f"xnb{k}", [128, D], BF16) for k in range(2)]
        P.dma("sp", self.lnG[i][:], g_ap.partition_broadcast(128))
        P.dma("sp", self.lnB[i][:], b_ap.partition_broadcast(128))
        P.dma("sp", self.lnGT[i][:], g_ap.rearrange("(c p) -> p c", p=128), allow_slow_non_contiguous=True)
        P.dma("sp", self.lnBT[i][:], b_ap.rearrange("(c p) -> p c", p=128), allow_slow_non_contiguous=True)
        return i

    def ln_tile(self, i, li, part=None, xb=None):
        P = self.P
        Xt = self.X[:, i, :]
        st, mv, r = self.lnst[i % 4], self.lnmv[i % 4], self.lnr[i % 4]
        if xb is None:
            xb = self.xnb[i % 2]
        if part == "b":
            return self.ln_tile_b(i, li, xb)
        for hlf in range(2):
            P.op("dve", lambda e, hlf=hlf: e.bn_stats(out=st[:, hlf, :], in_=self.X[:, i, hlf * 512:(hlf + 1) * 512]),
                 reads=[self.X[:, i, hlf * 512:(hlf + 1) * 512]], writes=[st[:, hlf, :]])
        P.op("dve", lambda e: e.bn_aggr(out=mv[:], in_=st[:]), reads=[st[:]], writes=[mv[:]])
        import os
        dbg = int(os.environ.get("DBG_LN", "9"))
        if dbg < 2:
            return
        P.act(r[:, 0:1], mv[:, 1:2], AF.Ln, bias=self.eps_t[:, 0:1])
        P.act(r[:, 1:2], r[:, 0:1], AF.Exp, scale=-0.5)
        if dbg < 3:
            return
        P.ts(Xt, Xt, mv[:, 0:1], ALU.subtract, r[:, 1:2], ALU.mult)
        if dbg < 4:
            return
        self.tile_ctr += 1
        P.copy(xb[:], Xt, eng="act")
        if dbg < 5:
            return
        P.tt(Xt, Xt, self.lnG[li][:], ALU.mult, eng="pool")
        P.tt(Xt, Xt, self.lnB[li][:], ALU.add, eng="pool")
        if dbg < 6 or part == "a":
            return
        self.ln_tile_b(i, li, xb)

    def ln_tile_b(self, i, li, xb):
        P = self.P
        dbg = 9
        pb = P.rr(2) // 2
        pts = [P.psb[:, (2 * pb + c % 2) * 1024 + (c // 2) * 128:(2 * pb + c % 2) * 1024 + (c // 2) * 128 + 128]
               for c in range(8)]
        for c in range(8):
            P.tr(pts[c], xb[:, c * 128:(c + 1) * 128], self.ident[:])
        if dbg < 7:
            return
        for c in range(8):
            dst = self.XT[:, c, i * 128:(i + 1) * 128]
            src = pts[c]
            if (c % 2 == 0 or dbg == 7) and dbg != 8:
                P.ts(dst, src, self.lnGT[li][:, c:c + 1], ALU.mult, self.lnBT[li][:, c:c + 1], ALU.add)
            else:
                P.act(dst, src, AF.Identity, bias=self.lnBT[li][:, c:c + 1], scale=self.lnGT[li][:, c:c + 1])

    def phase_input(self):
        P = self.P
        m = P.mark()
        li = self.load_ln(self.prm_("ln_in_g"), self.prm_("ln_in_b"))
        def one(i):
            P.dma("sp", self.X[:, i, :], self.x_d[i * 128:(i + 1) * 128, :])
            self.ln_tile(i, li)
        for i in range(0, NT, 2):
            P.zipped([lambda i=i: one(i), lambda i=i: one(i + 1)])
        P.release(m)
        self.tap("xt0", self.XT[:, :, 0:256], [128, 8, 256], BF16)

    def layer(self, l):
        P = self.P
        for i in range(NT):
            P.dma("sp", self.xs_d[i * 128:(i + 1) * 128, :], self.X[:, i, :])
        P.x_live = False
        m0 = P.mark()
        xbase = P.base[self.X.name][1]
        ybase = xbase + 64 * 1024 - 2 * 4 * S * 2
        self.y_retT = P.alloc_at("y_retT", [128, 4, S], BF16, ybase)
        self.y_rwkvT = P.alloc_at("y_rwkvT", [128, 4, S], BF16, ybase + 4 * S * 2)
        P.arena_hi_save = P.arena_hi
        P.arena_hi = ybase
        if "ret1h" in self.taps:
            P.memset(self.y_retT[:], 0.0)
        if "rwkv_short" in self.taps:
            P.memset(self.y_rwkvT[:], 0.0)
        self.Wr = P.alloc("Wr", [128, 8, 1792], BF16)

        def issue_wr():
            w_in = self.prm_("w_in")
            for j in range(7):
                src = w_in[l, :, 2048 + j * 256:2048 + (j + 1) * 256].rearrange("(c p) n -> p c n", p=128)
                P.dma("pool", self.Wr[:, :, j * 256:(j + 1) * 256], src)
        self.issue_wr = issue_wr
        self.phase_ret(l)
        if self.issue_wr is not None:
            self.issue_wr()
            self.issue_wr = None
        self.tap("yretT", self.y_retT[:], [128, 4, S], BF16)
        if self.stage == "ret":
            return
        self.wru = P.alloc("wru", [128, 4, D], BF16)
        self.wwu = P.alloc("wwu", [128, 4, D], BF16)

        def issue_wu():
            P.dma("pool", self.wru[:], self.prm_("w_ret_up")[l].rearrange("(c p) n -> p c n", p=128))
            P.dma("pool", self.wwu[:], self.prm_("w_rwkv_up")[l].rearrange("(c p) n -> p c n", p=128))
        self.issue_wu = issue_wu
        self.phase_rwkv(l)
        self.tap("yrwkvT", self.y_rwkvT[:], [128, 4, S], BF16)
        if self.stage == "rwkv":
            return
        self.mergedT = P.alloc("mergedT", [128, 8, S], BF16)
        self.wo_buf = P.alloc("w_out", [128, 8, D], BF16)
        self.phase_merge(l)
        self.tap("mergedT", self.mergedT[:, :, 0:512], [128, 8, 512], BF16)
        if self.stage == "merge":
            return
        P.arena_hi = P.arena_hi_save
        P.x_live = True
        ptr_save = P.ptr
        P.ptr = P.base[self.Wr.name][1]
        self.phase_out(l)
        assert P.ptr <= P.base[self.mergedT.name][1]
        P.ptr = ptr_save
        P.release(m0)
        self.tap(f"x1_{l}", self.X[:], [128, NT, D], F32)
        if self.stage == "x1":
            return
        self.phase_xattn(l)
        self.tap(f"x2_{l}", self.X[:], [128, NT, D], F32)
        if self.stage == "x2":
            return
        self.phase_moe(l)
        self.tap(f"x3_{l}", self.X[:], [128, NT, D], F32)

    def phase_merge(self, l):
        P = self.P
        m = P.mark()
        w_in = self.prm_("w_in")
        wru, wwu = self.wru, self.wwu
        Wg = [P.alloc(f"Wg{i}", [128, 8, 256], BF16) for i in range(2)]
        sgr = [P.alloc(f"sgr{i}", [128, 512], F32) for i in range(2)]
        sgw = [P.alloc(f"sgw{i}", [128, 512], F32) for i in range(2)]

        def load_g(cc):
            W = Wg[cc % 2]
            for j in range(2):
                c0 = 3840 + j * 1024 + cc * 128
                P.dma("pool", W[:, :, j * 128:(j + 1) * 128], w_in[l, :, c0:c0 + 128].rearrange("(c p) n -> p c n", p=128))
        load_g(0)
        k = 0
        for cc in range(8):
            if cc + 1 < 8:
                load_g(cc + 1)
            if cc == 3:
                for j in range(2):
                    P.dma("pool", self.wo_buf[:, :, j * 512:(j + 1) * 512],
                          self.prm_("w_out")[l, :, j * 512:(j + 1) * 512].rearrange("(c p) n -> p c n", p=128))
            W = Wg[cc % 2]
            csl = slice(cc * 128, (cc + 1) * 128)
            for tg in range(4):
                tsl = slice(tg * 512, (tg + 1) * 512)
                pgr = P.bank(P.rr())
                for c in range(8):
                    P.mm(pgr, W[:, c, 0:128], self.XT[:, c, tsl], start=(c == 0), stop=(c == 7))
                pgw = P.bank(P.rr())
                for c in range(8):
                    P.mm(pgw, W[:, c, 128:256], self.XT[:, c, tsl], start=(c == 0), stop=(c == 7))
                pur = P.bank(P.rr())
                for c in range(4):
                    P.mm(pur, wru[:, c, csl], self.y_retT[:, c, tsl], start=(c == 0), stop=(c == 3))
                puw = P.bank(P.rr())
                for c in range(4):
                    P.mm(puw, wwu[:, c, csl], self.y_rwkvT[:, c, tsl], start=(c == 0), stop=(c == 3))
                a, b = sgr[k % 2], sgw[k % 2]
                k += 1
                P.act(a[:], pgr, AF.Sigmoid)
                P.act(b[:], pgw, AF.Sigmoid)
                P.tt(a[:], a[:], pur, ALU.mult)
                P.tt(b[:], b[:], puw, ALU.mult)
                P.tt(self.mergedT[:, cc, tsl], a[:], b[:], ALU.add)
        P.release(m)

    def phase_out(self, l):
        P = self.P
        m = P.mark()
        wo = self.wo_buf
        xin = [P.alloc(f"xin{i}", [128, D], F32) for i in range(2)]
        li = self.load_ln(self.prm_("ln1_g")[l], self.prm_("ln1_b")[l])
        assert P.ptr <= P.base[self.mergedT.name][1], "phase_out scratch runs into mergedT"
        def one(i):
            isl = slice(i * 128, (i + 1) * 128)
            P.dma("sp", xin[i % 2][:], self.xs_d[isl, :])
            b = P.rr(2)
            ph = P.bank(b, 2)
            for hf in range(2):
                for c in range(8):
                    P.mm(ph[:, hf * 512:(hf + 1) * 512], self.mergedT[:, c, isl], wo[:, c, hf * 512:(hf + 1) * 512],
                         start=(c == 0), stop=(c == 7))
            P.stt(self.X[:, i, :], xin[i % 2][:], ALPHA, ph, ALU.mult, ALU.add)
            self.ln_tile(i, li)
        for i in range(0, NT, 2):
            P.zipped([lambda i=i: one(i), lambda i=i: one(i + 1)])
        P.release(m)

    def phase_mem(self):
        P = self.P
        self.memT = P.alloc("memT", [128, 8, 256], BF16)
        m = P.mark()
        mt = P.alloc("memtmp", [128, D], F32)
        mb = P.alloc("memb", [128, D], BF16)
        for t in range(2):
            P.dma("sp", mt[:], self.mem_d[t * 128:(t + 1) * 128, :])
            P.copy(mb[:], mt[:], eng="act")
            pt = P.bankb(P.rr())
            for c in range(8):
                P.tr(pt[:, c * 128:(c + 1) * 128], mb[:, c * 128:(c + 1) * 128], self.ident[:])
            P.copy(self.memT[:, :, t * 128:(t + 1) * 128], pt.rearrange("p (c k) -> p c k", k=128))
        P.release(m)

    def phase_xattn(self, l):
        P = self.P
        m = P.mark()
        kT = P.alloc("kT", [128, 8, 256], BF16)
        vtok = P.alloc("vtokx", [128, 2, D], BF16)
        qT = P.alloc("qTx", [128, 8, S], BF16)
        wkv = self.prm_("xa_wkv")
        wA = P.alloc("wxa", [128, 8, D], BF16)
        wB = P.alloc("wxb", [128, 8, D], BF16)
        m1 = P.mark()
        wk = wA
        for j in range(2):
            P.dma("pool", wk[:, :, j * 512:(j + 1) * 512],
                  wkv[l, :, j * 512:(j + 1) * 512].rearrange("(c p) n -> p c n", p=128))
        wv = wB
        for j in range(2):
            P.dma("pool", wv[:, :, j * 512:(j + 1) * 512],
                  wkv[l, :, D + j * 512:D + (j + 1) * 512].rearrange("(c p) n -> p c n", p=128))
        for cc in range(8):
            pk = P.bank(P.rr())[:, 0:256]
            for c in range(8):
                P.mm(pk, wk[:, c, cc * 128:(cc + 1) * 128], self.memT[:, c, :], start=(c == 0), stop=(c == 7))
            P.copy(kT[:, cc, :], pk, eng="act")
        P.release(m1)
        wq = wA
        for j in range(2):
            P.dma("pool", wq[:, :, j * 512:(j + 1) * 512],
                  self.prm_("xa_wq")[l, :, j * 512:(j + 1) * 512].rearrange("(c p) n -> p c n", p=128))
        for mt in range(2):
            for hf in range(2):
                pv = P.bank(P.rr())
                for c in range(8):
                    P.mm(pv, self.memT[:, c, mt * 128:(mt + 1) * 128], wv[:, c, hf * 512:(hf + 1) * 512],
                         start=(c == 0), stop=(c == 7))
                P.copy(vtok[:, mt, hf * 512:(hf + 1) * 512], pv, eng="act")
        P.release(m1)
        wo = wB
        for j in range(2):
            P.dma("pool", wo[:, :, j * 512:(j + 1) * 512],
                  self.prm_("xa_wo")[l, :, j * 512:(j + 1) * 512].rearrange("(c p) n -> p c n", p=128))
        k = 0
        for cc in range(8):
            for tg in range(4):
                tsl = slice(tg * 512, (tg + 1) * 512)
                pq = P.bank(P.rr())
                for c in range(8):
                    P.mm(pq, wq[:, c, cc * 128:(cc + 1) * 128], self.XT[:, c, tsl], start=(c == 0), stop=(c == 7))
                if k % 2 == 0:
                    P.act(qT[:, cc, tsl], pq, AF.Copy, scale=1.0 / 16.0)
                else:
                    P.ts(qT[:, cc, tsl], pq, 1.0 / 16.0, ALU.mult)
                k += 1
        P.release(m1)
        li = self.load_ln(self.prm_("ln2_g")[l], self.prm_("ln2_b")[l])
        P.ts(self.lnG[li][:], self.lnG[li][:], ALPHA, ALU.mult, eng="pool")
        P.ts(self.lnB[li][:], self.lnB[li][:], ALPHA, ALU.mult, eng="pool")
        mxs = [P.alloc(f"mx{i}", [128, 4], F32) for i in range(2)]
        nmxs = [P.alloc(f"nmx{i}", [128, 4], F32) for i in range(2)]
        ssums = [P.alloc(f"ssum{i}", [128, 4], F32) for i in range(2)]
        rss = [P.alloc(f"rsx{i}", [128, 4], F32) for i in range(2)]
        pe_ = [P.alloc(f"pe{i}", [128, 4, 256], BF16) for i in range(2)]
        pT = [P.alloc(f"pT{i}", [128, 8, 128], BF16) for i in range(2)]
        oT = [P.alloc(f"oT{i}", [128, 8, 128], BF16) for i in range(2)]

        def one(i):
            mx, nmx, ssum, rs = mxs[i % 2], nmxs[i % 2], ssums[i % 2], rss[i % 2]
            isl = slice(i * 128, (i + 1) * 128)
            b = P.rr(2)
            ps = P.bank(b, 2)
            for h in range(4):
                for dc in range(2):
                    P.mm(ps[:, h * 256:(h + 1) * 256], qT[:, 2 * h + dc, isl], kT[:, 2 * h + dc, :],
                         start=(dc == 0), stop=(dc == 1))
            P.reduce(mx[:], ps.rearrange("p (h m) -> p h m", h=4), ALU.max)
            P.ts(nmx[:], mx[:], -1.0, ALU.mult)
            pe = pe_[i % 2]
            for h in range(4):
                P.act(pe[:, h, :], ps[:, h * 256:(h + 1) * 256], AF.Exp, bias=nmx[:, h:h + 1],
                      accum_out=ssum[:, h:h + 1])
            P.recip(rs[:], ssum[:])
            P.tt(pe[:], pe[:], _bc(rs[:].unsqueeze(2), [128, 4, 256]), ALU.mult)
            ppT = P.bankb(P.rr())
            for h in range(4):
                for mt in range(2):
                    j = h * 2 + mt
                    P.tr(ppT[:, j * 128:(j + 1) * 128], pe[:, h, mt * 128:(mt + 1) * 128], self.ident[:])
            P.copy(pT[i % 2][:].rearrange("p a b -> p (a b)"), ppT, eng="act")
            b = P.rr(2)
            po = P.bank(b, 2)
            for h in range(4):
                for ec in range(2):
                    j = h * 2 + ec
                    for mt in range(2):
                        P.mm(po[:, j * 128:(j + 1) * 128], vtok[:, mt, h * 256 + ec * 128:h * 256 + (ec + 1) * 128],
                             pT[i % 2][:, h * 2 + mt, :], start=(mt == 0), stop=(mt == 1))
            P.copy(oT[i % 2][:].rearrange("p a b -> p (a b)"), po)
            b = P.rr(2)
            ph = P.bank(b, 2)
            for hf in range(2):
                for c in range(8):
                    P.mm(ph[:, hf * 512:(hf + 1) * 512], oT[i % 2][:, c, :], wo[:, c, hf * 512:(hf + 1) * 512],
                         start=(c == 0), stop=(c == 7))
            P.stt(self.X[:, i, :], self.X[:, i, :], ALPHA, ph, ALU.mult, ALU.add)
            self.ln_tile(i, li)
        for i in range(0, NT, 2):
            P.zipped([lambda i=i: one(i), lambda i=i: one(i + 1)])
        P.release(m)

    def phase_moe(self, l):
        P = self.P
        m = P.mark()
        XT = self.XT
        wg = [P.alloc(f"wg{i}", [128, 8, 512], BF16) for i in range(2)]
        wu = [P.alloc(f"wu{i}", [128, 8, 512], BF16) for i in range(2)]
        wd = [P.alloc(f"wd{i}", [128, 4, D], BF16) for i in range(2)]
        hT = [P.alloc(f"hT{i}", [128, 4, 512], BF16) for i in range(2)]
        sgl = [P.alloc(f"sgl{i}", [128, 512], F32) for i in range(2)]
        mg, mu_, md = self.prm_("moe_w_gate"), self.prm_("moe_w_up"), self.prm_("moe_w_down")

        def load_e(e):
            P.dma("pool", wg[e % 2][:], mg[l, e].rearrange("(c p) n -> p c n", p=128))
            P.dma("pool", wu[e % 2][:], mu_[l, e].rearrange("(c p) n -> p c n", p=128))
            for j in range(2):
                P.dma("pool", wd[e % 2][:, :, j * 512:(j + 1) * 512],
                      md[l, e, :, j * 512:(j + 1) * 512].rearrange("(c p) n -> p c n", p=128))
        load_e(0)
        rw = P.alloc("rw", [128, 8, 16], BF16)
        P.dma("pool", rw[:], self.prm_("router_w").rearrange("(c p) e -> p c e", p=128))
        rb = P.alloc("rb", [128, 16], F32)
        P.dma("sp", rb[:], self.prm_("router_bias").partition_broadcast(128))

        def R(name, n=256):
            return P.alloc(name, [128, n], F32)
        aff, ch, t_, mc, sel1, sel2, w_ = R("aff"), R("ch"), R("t_"), R("mc"), R("sel1"), R("sel2"), R("w_")
        comb = R("comb")
        ps6 = R("ps6", 64 * 6)
        gs, ing, pen = R("gs", 64), R("ing", 64), R("pen", 64)
        gmax, m1_, m2_, wsum, rws = R("gmax", 16), R("m1", 16), R("m2", 16), R("wsum", 16), R("rws", 16)
        pl = P.bank(P.rr())
        for i in range(NT):
            for c in range(8):
                P.mm(pl[:, i * 16:(i + 1) * 16], XT[:, c, i * 128:(i + 1) * 128], rw[:, c, :], start=(c == 0), stop=(c == 7))
        P.act(aff[:], pl[:, 0:256], AF.Sigmoid)
        v3 = lambda t: t[:].rearrange("p (a e) -> p a e", e=16)
        g3 = lambda t: t[:].rearrange("p (a e) -> p a e", e=4)
        P.tt(v3(ch), v3(aff), _bc(rb[:].unsqueeze(1), [128, 16, 16]), ALU.add)
        c4 = g3(ch)
        p6 = ps6[:].rearrange("p (a k) -> p a k", k=6)
        P.tt(p6[:, :, 0:3], _bc(c4[:, :, 0:1], [128, 64, 3]), c4[:, :, 1:4], ALU.add)
        P.tt(p6[:, :, 3:5], _bc(c4[:, :, 1:2], [128, 64, 2]), c4[:, :, 2:4], ALU.add)
        P.tt(p6[:, :, 5:6], c4[:, :, 2:3], c4[:, :, 3:4], ALU.add)
        P.reduce(gs[:], p6, ALU.max)
        gs3 = gs[:].rearrange("p (a g) -> p a g", g=4)
        P.reduce(gmax[:], gs3, ALU.max)
        P.tt(ing[:].rearrange("p (a g) -> p a g", g=4), gs3, _bc(gmax[:].unsqueeze(2), [128, 16, 4]), ALU.is_equal)
        P.ts(pen[:], ing[:], 1.0, ALU.subtract, 1e30, ALU.mult)
        P.tt(g3(t_), c4, _bc(ing[:].unsqueeze(2), [128, 64, 4]), ALU.mult)
        P.tt(g3(mc), g3(t_), _bc(pen[:].unsqueeze(2), [128, 64, 4]), ALU.add)
        P.reduce(m1_[:], v3(mc), ALU.max)
        P.tt(v3(sel1), v3(mc), _bc(m1_[:].unsqueeze(2), [128, 16, 16]), ALU.is_equal)
        P.stt(t_[:], sel1[:], -1e30, mc[:], ALU.mult, ALU.add)
        P.reduce(m2_[:], v3(t_), ALU.max)
        P.tt(v3(sel2), v3(t_), _bc(m2_[:].unsqueeze(2), [128, 16, 16]), ALU.is_equal)
        P.tt(sel1[:], sel1[:], sel2[:], ALU.add)
        P.tt(w_[:], aff[:], sel1[:], ALU.mult)
        P.reduce(wsum[:], v3(w_), ALU.add)
        P.recip(rws[:], wsum[:])
        P.tt(v3(comb), v3(w_), _bc(rws[:].unsqueeze(2), [128, 16, 16]), ALU.mult)
        self.tap(f"comb{l}", comb[:], [128, 256], F32)
        ne = 16
        k = 0
        li = self.load_ln(self.prm_("ln3_g")[l], self.prm_("ln3_b")[l])
        xnb4 = [P.alloc(f"xnb4_{i}", [128, D], BF16) for i in range(4)]
        pending = []
        for e in range(ne):
            if e + 1 < ne:
                load_e(e + 1)
            g_, u_, d_ = wg[e % 2], wu[e % 2], wd[e % 2]
            for tg in range(4):
                tsl = slice(tg * 512, (tg + 1) * 512)
                h_ = hT[(e * 4 + tg) % 2]
                for fc in range(4):
                    pg = P.bank(P.rr())
                    for c in range(8):
                        P.mm(pg, g_[:, c, fc * 128:(fc + 1) * 128], XT[:, c, tsl], start=(c == 0), stop=(c == 7))
                    pu = P.bank(P.rr())
                    for c in range(8):
                        P.mm(pu, u_[:, c, fc * 128:(fc + 1) * 128], XT[:, c, tsl], start=(c == 0), stop=(c == 7))
                    sg_ = sgl[k % 2]
                    k += 1
                    P.act(sg_[:], pg, AF.Silu)
                    P.tt(h_[:, fc, :], sg_[:], pu, ALU.mult)
                for i in pending:
                    self.ln_tile(i, li, part="b", xb=xnb4[i % 4])
                pending = []
                for ti in range(4):
                    i = tg * 4 + ti
                    b = P.rr(2)
                    pd = P.bank(b, 2)
                    for hf in range(2):
                        for fc in range(4):
                            P.mm(pd[:, hf * 512:(hf + 1) * 512], h_[:, fc, ti * 128:(ti + 1) * 128],
                                 d_[:, fc, hf * 512:(hf + 1) * 512], start=(fc == 0), stop=(fc == 3))
                    P.stt(self.X[:, i, :], pd, comb[:, i * 16 + e:i * 16 + e + 1], self.X[:, i, :], ALU.mult, ALU.add)
                if e == ne - 1:
                    for ti in range(4):
                        i = tg * 4 + ti
                        self.ln_tile(i, li, part="a", xb=xnb4[i % 4])
                        pending.append(i)
        for i in pending:
            self.ln_tile(i, li, part="b", xb=xnb4[i % 4])
        P.release(m)

    def phase_rwkv(self, l):
        P = self.P
        m = P.mark()
        import os
        dbgrw = int(os.environ.get("DBG_RW", "99"))
        TB = 128
        NCH = TB // 64
        NB = S // TB
        if "rwkv_short" in self.taps:
            NB = 2
        w_in = self.prm_("w_in")
        Wr = self.Wr
        waup = P.alloc("waup", [128, 512], BF16)
        P.dma("pool", waup[0:64, :], self.prm_("rwkv_w_up")[l])
        P.dma("pool", waup[64:128, :], self.prm_("rwkv_a_up")[l])
        gup = P.alloc("gup", [128, 512], BF16)
        P.dma("pool", gup[:], self.prm_("rwkv_g_up")[l])
        self.issue_wu()
        cols = P.alloc("cols", [128, 7, 4], F32)
        names = ["rwkv_w0", "rwkv_a0", "rwkv_k_k", "rwkv_k_a", None, "rwkv_ln_g", "rwkv_ln_b"]
        for idx, nm in enumerate(names):
            if nm is None:
                src = self.prm_("rwkv_r_k")[l].rearrange("h d -> (h d)")
            else:
                src = self.prm_(nm)[l]
            P.dma("sp", cols[:, idx, :], src.rearrange("(a p) -> p a", p=128), allow_slow_non_contiguous=True)
        muT = P.alloc("muT", [128, 14], F32)
        P.dma("sp", muT[:], self.prm_("rwkv_mu")[l].rearrange("(a p) -> p a", p=128), allow_slow_non_contiguous=True)
        wmask1 = P.alloc("wmask1", [64, 16, 128], BF16)
        P.dma("sp", wmask1[:], self.cst("wmask1"))
        wmask2 = P.alloc("wmask2", [64, 8, 64], BF16)
        P.dma("sp", wmask2[:], self.cst("wmask2"))
        identrep = P.alloc("identrep", [64, 8, 64], BF16)
        P.dma("sp", identrep[:], self.cst("identrep"))
        rstm = P.alloc("rstm", [128, 4, TB], F32)
        P.dma("sp", rstm[:], self.cst("rstmask")[:, :, 0:TB])
        epsg = P.alloc("epsg", [128, 1], F32)
        P.memset(epsg[:], 64e-5)

        def A4(name, dt=F32):
            return P.alloc(name, [128, 4, TB], dt)

        def D2(name, dt=F32):
            a = A4(name, dt)
            return [a, a]
        praw = [P.alloc(f"praw{i}", [128, TB + 1], F32) for i in range(2)]
        dtmp = [P.alloc(f"dtmp{i}", [128, TB], F32) for i in range(2)]
        r_, k_, v_ = A4("r"), A4("k"), A4("v")
        dwda = P.alloc("dwda", [128, TB], F32)
        dg = P.alloc("dg", [128, TB], F32)
        th = P.alloc("th", [128, TB], BF16)
        sgd = P.alloc("sgd", [128, TB], BF16)
        sig, Lsig, Lm, emL = A4("sig"), A4("Lsig"), A4("Lm"), A4("emL")
        a_, kk, kkn, k2, tmp, rn = A4("a"), A4("kk"), A4("kkn"), A4("k2"), A4("tmp"), A4("rn")
        sq = A4("sq", BF16)
        rkb = A4("rkb", BF16)
        eL_ = D2("eL")
        g__ = D2("g", BF16)
        vb_ = D2("vb", BF16)
        bonus_ = [sig, sig]
        AR_ = [P.alloc("AR", [128, 4, NCH, 2, 64], BF16)] * 2
        BK_ = [P.alloc("BK", [128, 4, NCH, 2, 64], BF16)] * 2
        BKe_ = [P.alloc("BKe", [128, 4, NCH, 2, 64], BF16)] * 2
        YT = Lsig
        YTb = A4("YTb", BF16)
        sqp = A4("sqp", BF16)
        rnp = A4("rnp")
        cen = YT
        Vtok = [P.alloc(f"Vtok{i}", [64, 512], BF16) for i in range(2)]
        Btok = [P.alloc(f"Btok{i}", [64, 1024], BF16) for i in range(2)]
        AM = [P.alloc(f"AM{i}", [64, 8, 2, 128], BF16) for i in range(2)]
        Qb = [P.alloc(f"Qb{i}", [64, NCH * 8, 64], BF16) for i in range(2)]
        Pb = [P.alloc(f"Pb{i}", [64, NCH * 8, 64], BF16) for i in range(2)]
        Xb = [P.alloc(f"Xb{i}", [64, NCH * 8, 64], BF16) for i in range(2)]
        G2 = P.alloc("G2", [64, 512], F32)
        Y2 = P.alloc("Y2", [64, 512], F32)
        Gs = P.alloc("Gs", [64, 512], BF16)
        Us = P.alloc("Us", [64, 512], BF16)
        Yt = P.alloc("Yt", [64, 512], F32)
        Ytb = P.alloc("Ytb", [64, 512], BF16)
        T = P.alloc("T", [128, 4, 64], F32)
        Tb = [P.alloc(f"Tb{i}", [128, 4, 64], BF16) for i in range(2)]
        P.memset(T[:], 0.0)
        P.memset(Tb[0][:], 0.0)
        XT = self.XT
        blk = self.blkones
        ident = self.ident

        def f2(t):
            return t[:].rearrange("p a b -> p (a b)")

        def v4(t):
            return t[:].rearrange("p a (n t) -> p a n t", t=64)

        def front(tb, part):
            q = tb % 2
            eL, g_, vb, bonus, AR, BK, BKe = eL_[q], g__[q], vb_[q], bonus_[q], AR_[q], BK_[q], BKe_[q]
            t0 = tb * TB
            lo = 0 if tb > 0 else 1
            for j in range(14 if part == 0 else 0):
                pa = P.bank(P.rr())
                for c in range(8):
                    P.mm(pa[:, lo:TB + 1], Wr[:, c, j * 128:(j + 1) * 128], XT[:, c, t0 - 1 + lo:t0 + TB],
                         start=(c == 0), stop=(c == 7))
                pr = praw[j % 2]
                P.copy(pr[:, lo:TB + 1], pa[:, lo:TB + 1], eng="act")
                if tb == 0:
                    P.memset(pr[:, 0:1], 0.0)
                d = dtmp[j % 2]
                P.tt(d[:], pr[:, 0:TB], pa[:, 1:TB + 1], ALU.subtract)
                if j < 4:
                    dst = r_[:, j, :]
                elif j < 8:
                    dst = k_[:, j - 4, :]
                elif j < 12:
                    dst = v_[:, j - 8, :]
                elif j == 12:
                    dst = dwda[:]
                else:
                    dst = dg[:]
                P.stt(dst, d[:], muT[:, j:j + 1], pa[:, 1:TB + 1], ALU.mult, ALU.add)
            if part == 0:
                return
            P.act(th[0:64, :], dwda[0:64, :], AF.Tanh)
            P.copy(th[64:128, :], dwda[64:128, :], eng="act")
            P.act(sgd[:], dg[:], AF.Sigmoid)
            pz = P.bank(P.rr())
            for p in range(4):
                P.mm(pz[:, p * TB:(p + 1) * TB], waup[0:64, p * 128:(p + 1) * 128], th[0:64, :])
            for p in range(4):
                P.act(sig[:, p, :], pz[:, p * TB:(p + 1) * TB], AF.Sigmoid, bias=cols[:, 0, p:p + 1])
            pa_ = P.bank(P.rr())
            for p in range(4):
                P.mm(pa_[:, p * TB:(p + 1) * TB], waup[64:128, p * 128:(p + 1) * 128], th[64:128, :])
            for p in range(4):
                P.act(a_[:, p, :], pa_[:, p * TB:(p + 1) * TB], AF.Sigmoid, bias=cols[:, 1, p:p + 1])
            pg = P.bank(P.rr())
            for p in range(4):
                P.mm(pg[:, p * TB:(p + 1) * TB], gup[:, p * 128:(p + 1) * 128], sgd[:])
            P.copy(f2(g_), pg[:, 0:4 * TB], eng="act")
            P.op("dve", lambda e: e.tensor_tensor_scan(out=f2(Lsig), data0=f2(rstm), data1=f2(sig), initial=0.0,
                                                       op0=ALU.mult, op1=ALU.add),
                 reads=[f2(rstm), f2(sig)], writes=[f2(Lsig)])
            P.tt(f2(Lm), f2(Lsig), f2(sig), ALU.subtract)
            P.act(f2(eL), f2(Lsig), AF.Exp, scale=-C0)
            P.act(f2(emL), f2(Lsig), AF.Exp, scale=C0)
            P.act(f2(Lm), f2(Lm), AF.Exp, scale=-C0)
            P.tt(kk[:], k_[:], _bc(cols[:, 2, :].unsqueeze(2), [128, 4, TB]), ALU.mult)
            P.tt(f2(sq), f2(kk), f2(kk), ALU.mult)
            pss = P.bank(P.rr())
            for p in range(4):
                P.mm(pss[:, p * TB:(p + 1) * TB], blk[:], sq[:, p, :])
            P.ts(f2(rn), pss[:, 0:4 * TB], 1e-19, ALU.max)
            P.act(f2(rn), f2(rn), AF.Ln)
            P.act(f2(rn), f2(rn), AF.Exp, scale=-0.5)
            P.tt(f2(kkn), f2(kk), f2(rn), ALU.mult)
            P.ts(f2(kk), f2(kkn), -1.0, ALU.mult)
            P.ts(f2(tmp), f2(a_), 1.0, ALU.subtract)
            P.tt(tmp[:], tmp[:], _bc(cols[:, 3, :].unsqueeze(2), [128, 4, TB]), ALU.mult)
            P.stt(f2(k2), f2(tmp), 1.0, f2(k_), ALU.add, ALU.mult)
            P.tt(AR[:, :, :, 0, :], v4(kk), v4(Lm), ALU.mult)
            P.tt(AR[:, :, :, 1, :], v4(r_), v4(eL), ALU.mult)
            P.tt(f2(tmp), f2(kkn), f2(a_), ALU.mult)
            P.tt(BK[:, :, :, 0, :], v4(tmp), v4(emL), ALU.mult)
            P.tt(BK[:, :, :, 1, :], v4(k2), v4(emL), ALU.mult)
            for p in range(4):
                for n in range(NCH):
                    P.ts(BKe[:, p, n, :, :], BK[:, p, n, :, :], eL[:, p, n * 64 + 63:n * 64 + 64], ALU.mult)
            P.tt(f2(tmp), f2(r_), f2(k2), ALU.mult)
            P.tt(rkb[:], tmp[:], _bc(cols[:, 4, :].unsqueeze(2), [128, 4, TB]), ALU.mult)
            pbs = P.bank(P.rr())
            for p in range(4):
                P.mm(pbs[:, p * TB:(p + 1) * TB], blk[:], rkb[:, p, :])
            P.tt(f2(bonus), pbs[:, 0:4 * TB], f2(v_), ALU.mult)
            P.copy(f2(vb), f2(v_), eng="act")

        def rear(tb, part):
            q = tb % 2
            eL, g_, vb, bonus, AR, BK, BKe = eL_[q], g__[q], vb_[q], bonus_[q], AR_[q], BK_[q], BKe_[q]
            t0 = tb * TB
            NU = NCH * 8
            for n in range(NCH if part == 0 else 0):
                ns = slice(n * 64, (n + 1) * 64)
                b2 = P.rr(2)
                ptv = P.bankb(b2)
                ptk = P.bankb(b2 + 1)
                for p in range(4):
                    P.tr(ptv[0:64, p * 128:(p + 1) * 128], vb[:, p, ns], ident[:])
                for p in range(4):
                    P.tr(ptk[0:64, p * 128:(p + 1) * 128], BKe[:, p, n, 0, :], ident[:])
                    P.tr(ptk[0:64, 512 + p * 128:512 + (p + 1) * 128], BKe[:, p, n, 1, :], ident[:])
                P.copy(Vtok[n][:], ptv[0:64, 0:512], eng="act")
                P.copy(Btok[n][:], ptk[0:64, :])
                b4 = P.rr(4)
                pA = P.bank(b4, 4)
                for h in range(8):
                    p, j = h // 2, h % 2
                    js = slice(64 * j, 64 * j + 64)
                    o = (j * 4 + p) * 256
                    rhs = AR[js, p, n, :, :].rearrange("p a b -> p (a b)")
                    P.mm(pA[0:64, o:o + 128], BK[js, p, n, 0, :], rhs)
                    P.mm(pA[0:64, o + 128:o + 256], BK[js, p, n, 1, :], rhs)
                am = AM[n]
                P.tt(am[:].rearrange("t (p j) a b -> t j p (a b)", j=2),
                     pA[0:64, :].rearrange("t (j p c) -> t j p c", j=2, p=4),
                     wmask1[:].rearrange("t (j p a) b -> t j p (a b)", j=2, p=4, a=2), ALU.mult)
                bN = P.rr(2)
                pN = P.bank(bN, 2)
                for h in range(8):
                    p, j = h // 2, h % 2
                    js = slice(64 * j, 64 * j + 64)
                    o = j * 512 + p * 64
                    P.mm(pN[0:64, o:o + 64], AR[js, p, n, 0, :], BK[js, p, n, 0, :])
                P.tt(Qb[0][:, n * 8:(n + 1) * 8, :].rearrange("t (p j) b -> t j p b", j=2),
                     pN[0:64, :].rearrange("t (j x) -> t j x", j=2)[:, :, 0:256].rearrange("t j (p b) -> t j p b", b=64),
                     wmask2[:].rearrange("t (p j) b -> t j p b", j=2), ALU.mult)
                P.tt(Xb[0][:, n * 8:(n + 1) * 8, :], am[:, :, 0, 0:64], identrep[:], ALU.add)
            def P0u(u):
                return AM[u // 8][:, u % 8, 0, 0:64]
            Qc, Pc, Xc = Qb[0], None, Xb[0]
            fl = lambda t: t[:].rearrange("p h b -> p (h b)")
            for r in range(1, 7 if part == 0 else 1):
                Qn, Pn, Xn = Qb[r % 2], Pb[r % 2], Xb[r % 2] if r >= 2 else Xb[0]
                if r <= 5:
                    pQ = P.bank(P.rr(2), 2)
                    for u in range(NU):
                        Pu = P0u(u) if r == 1 else Pc[:, u, :]
                        P.mm(pQ[0:64, u * 64:(u + 1) * 64], Pu, Qc[:, u, :])
                if r <= 4:
                    pP = P.bank(P.rr(2), 2)
                    for u in range(NU):
                        Pu = P0u(u) if r == 1 else Pc[:, u, :]
                        P.mm(pP[0:64, u * 64:(u + 1) * 64], Qc[:, u, :], Pu)
                if r >= 2:
                    pX = P.bank(P.rr(2), 2)
                    for u in range(NU):
                        P.mm(pX[0:64, u * 64:(u + 1) * 64], Qc[:, u, :], Xc[:, u, :])
                if r <= 5:
                    P.copy(fl(Qn), pQ[0:64, 0:NU * 64], eng="act")
                if r <= 4:
                    P.copy(fl(Pn), pP[0:64, 0:NU * 64])
                if r >= 2:
                    P.tt(fl(Xn), pX[0:64, 0:NU * 64], fl(Xc), ALU.add)
                    Xc = Xn
                if r <= 5:
                    Qc = Qn
                if r <= 4:
                    Pc = Pn
            if part == 0:
                return
            XTf = Xb[0]
            for n in range(NCH):
                ns = slice(n * 64, (n + 1) * 64)
                am = AM[n]
                cc = n
                tcnt = tb * NCH + n
                pG2 = P.bank(P.rr())
                for h in range(8):
                    P.mm(pG2[0:64, h * 64:(h + 1) * 64], am[:, h, 1, 0:64], Vtok[cc][:, h * 64:(h + 1) * 64])
                P.copy(G2[:], pG2[0:64, :], eng="act")
                pY2 = P.bank(P.rr())
                for h in range(8):
                    P.mm(pY2[0:64, h * 64:(h + 1) * 64], am[:, h, 1, 64:128], Vtok[cc][:, h * 64:(h + 1) * 64])
                P.copy(Y2[:], pY2[0:64, :], eng="act")
                Tc, Tn = Tb[tcnt % 2], Tb[(tcnt + 1) % 2]
                pGA = P.bank(P.rr(2), 2)
                for h in range(8):
                    p, j = h // 2, h % 2
                    js = slice(64 * j, 64 * j + 64)
                    o = j * 512 + p * 64
                    P.mm(pGA[0:64, o:o + 64], AR[js, p, n, 0, :], Tc[js, p, :])
                P.tt(Gs[:].rearrange("t (p j v) -> t j p v", j=2, v=64),
                     pGA[0:64, :].rearrange("t (j x) -> t j x", j=2)[:, :, 0:256].rearrange("t j (p v) -> t j p v", v=64),
                     G2[:].rearrange("t (p j v) -> t j p v", j=2, v=64), ALU.add)
                pU = P.bank(P.rr())
                for h in range(8):
                    P.mm(pU[0:64, h * 64:(h + 1) * 64], XTf[:, n * 8 + h, :], Gs[:, h * 64:(h + 1) * 64])
                P.copy(Us[:], pU[0:64, :], eng="act")
                pYA = P.bank(P.rr(2), 2)
                for h in range(8):
                    p, j = h // 2, h % 2
                    js = slice(64 * j, 64 * j + 64)
                    o = j * 512 + p * 64
                    P.mm(pYA[0:64, o:o + 64], AR[js, p, n, 1, :], Tc[js, p, :])
                pYB = P.bank(P.rr())
                for h in range(8):
                    P.mm(pYB[0:64, h * 64:(h + 1) * 64], am[:, h, 0, 64:128], Us[:, h * 64:(h + 1) * 64])
                pTU = P.bank(P.rr())
                for p in range(4):
                    P.mm(pTU[:, p * 128:(p + 1) * 128], Btok[cc][:, p * 128:(p + 1) * 128], Us[:, p * 128:(p + 1) * 128],
                         start=True, stop=False)
                    P.mm(pTU[:, p * 128:(p + 1) * 128], Btok[cc][:, 512 + p * 128:512 + (p + 1) * 128],
                         Vtok[cc][:, p * 128:(p + 1) * 128], start=False, stop=True)
                for j in range(2):
                    js = slice(64 * j, 64 * j + 64)
                    P.tt(T[js, :, :], T[js, :, :], _bc(eL[js, :, n * 64 + 63:n * 64 + 64], [64, 4, 64]), ALU.mult)
                    tu = pTU[js, :].rearrange("p (a b) -> p a b", b=128)[:, :, 64 * j:64 * j + 64]
                    P.tt(T[js, :, :], T[js, :, :], tu, ALU.add)
                P.copy(Tn[:], T[:], eng="act")
                P.tt(Yt[:].rearrange("t (p j v) -> t j p v", j=2, v=64),
                     pYA[0:64, :].rearrange("t (j x) -> t j x", j=2)[:, :, 0:256].rearrange("t j (p v) -> t j p v", v=64),
                     Y2[:].rearrange("t (p j v) -> t j p v", j=2, v=64), ALU.add)
                P.tt(Ytb[:], pYB[0:64, :], Yt[:], ALU.add)
                pyt = P.bankb(P.rr())
                for p in range(4):
                    P.tr(pyt[:, p * 64:(p + 1) * 64], Ytb[:, p * 128:(p + 1) * 128], ident[0:64, 0:64])
                P.copy(YT[:, :, ns], pyt[:, 0:256].rearrange("p (a b) -> p a b", b=64), eng="act")
            P.copy(f2(YTb), f2(YT), eng="act")
            pm = P.bank(P.rr())
            for p in range(4):
                P.mm(pm[:, p * TB:(p + 1) * TB], blk[:], YTb[:, p, :])
            P.stt(f2(cen), pm[:, 0:4 * TB], -1.0 / 64.0, f2(YT), ALU.mult, ALU.add)
            P.act(f2(sqp), f2(cen), AF.Square)
            pv2 = P.bank(P.rr())
            for p in range(4):
                P.mm(pv2[:, p * TB:(p + 1) * TB], blk[:], sqp[:, p, :])
            P.act(f2(rnp), pv2[:, 0:4 * TB], AF.Ln, bias=epsg[:, 0:1], scale=1.0 / 64.0)
            P.act(f2(rnp), f2(rnp), AF.Exp, scale=-0.5)
            P.tt(f2(cen), f2(cen), f2(rnp), ALU.mult)
            P.tt(cen[:], cen[:], _bc(cols[:, 5, :].unsqueeze(2), [128, 4, TB]), ALU.mult)
            P.tt(cen[:], cen[:], _bc(cols[:, 6, :].unsqueeze(2), [128, 4, TB]), ALU.add)
            P.tt(f2(cen), f2(cen), f2(bonus), ALU.add)
            P.tt(self.y_rwkvT[:, :, t0:t0 + TB], cen[:], g_[:], ALU.mult)

        import os
        zg = int(os.environ.get("ZG", "0"))
        zs = int(os.environ.get("ZS", "6"))
        front(0, 0)
        for tb in range(NB):
            front(tb, 1)
            rear(tb, 0)
            if tb + 1 < NB and zg > 0:
                zsave = P.zip_gran
                P.zip_gran = zg
                P.zipped([lambda tb=tb: rear(tb, 1), lambda tb=tb: front(tb + 1, 0)], split=[zs, 8 - zs])
                P.zip_gran = zsave
            else:
                rear(tb, 1)
                if tb + 1 < NB:
                    front(tb + 1, 0)
        P.release(m)

    def phase_ret(self, l):
        P = self.P
        m = P.mark()
        cst = _get_consts()
        w_in = self.prm_("w_in")
        cos = P.alloc("cos", [128, S], BF16)
        sins = P.alloc("sins", [128, S], BF16)
        P.dma("sp", cos[:], self.cst("cos"))
        P.dma("sp", sins[:], self.cst("sins"))
        rmask = P.alloc("rmask", [128, 4, 128], F32)
        P.dma("sp", rmask[:], self.cst("rmask"))
        qdec = P.alloc("qdec", [128, 4, 512], BF16)
        P.dma("sp", qdec[:], self.cst("qdec"))
        kdec = P.alloc("kdec", [128, 4], F32)
        P.dma("sp", kdec[:], self.cst("kdec"))
        pswap = P.alloc("pswap", [128, 128], BF16)
        P.dma("sp", pswap[:], self.cst("pswap"))
        gng = P.alloc("gng", [128, 512], F32)
        P.dma("sp", gng[:], self.prm_("ret_gn_g")[l].partition_broadcast(128))
        Wh = [P.alloc(f"Wh{i}", [128, 8, 512], BF16) for i in range(2)]
        raw = [P.alloc(f"raw{i}", [128, 512], BF16) for i in range(2)]
        t1 = [P.alloc(f"t1_{i}", [128, 512], F32) for i in range(2)]
        t2 = [P.alloc(f"t2_{i}", [128, 512], F32) for i in range(2)]
        def per2(name, shape, dt):
            return [P.alloc(f"{name}{i}", shape, dt) for i in range(2)]
        qrot_, krot_, qd_ = per2("qrot", [128, S], BF16), per2("krot", [128, S], BF16), per2("qd", [128, S], BF16)
        vtok_, sg_, ktok_ = per2("vtok", [128, NT, 128], BF16), per2("sg", [128, NT, 128], BF16), per2("ktok", [128, NT, 128], BF16)
        state_ = per2("state", [128, 128], F32)
        stateb_ = [per2(f"stateb{i}_", [128, 128], BF16) for i in range(2)]
        scm_ = [per2(f"scm{i}_", [128, 128], BF16) for i in range(2)]
        yn_ = [per2(f"yn{i}_", [128, 128], F32) for i in range(2)]
        yb_ = [per2(f"yb{i}_", [128, 128], BF16) for i in range(2)]
        st1_, mv1_, r1_ = per2("st1", [128, 6], F32), per2("mv1", [128, 2], F32), per2("r1", [128, 2], F32)

        def load_w(h):
            W = Wh[h % 2]
            for j in range(4):
                src = w_in[l, :, j * 512 + h * 128:j * 512 + (h + 1) * 128].rearrange("(c p) n -> p c n", p=128)
                P.dma("pool", W[:, :, j * 128:(j + 1) * 128], src)
            return W

        nh = 1 if self.stage == "ret" and "ret1h" in self.taps else 4

        def front(h):
            W = Wh[h % 2]
            qrot, krot, qd = qrot_[h % 2], krot_[h % 2], qd_[h % 2]
            vtok, sg, ktok = vtok_[h % 2], sg_[h % 2], ktok_[h % 2]
            for j, dst in ((0, qrot), (1, krot)):
                for tg in range(4):
                    tsl = slice(tg * 512, (tg + 1) * 512)
                    k2 = (j * 4 + tg) % 2
                    pa = P.bank(P.rr())
                    for c in range(8):
                        P.mm(pa, W[:, c, j * 128:(j + 1) * 128], self.XT[:, c, tsl], start=(c == 0), stop=(c == 7))
                    P.copy(raw[k2][:], pa, eng="act")
                    pb_ = P.bank(P.rr())
                    P.mm(pb_, pswap[:], raw[k2][:])
                    P.tt(t1[k2][:], pa, cos[:, tsl], ALU.mult)
                    P.tt(t2[k2][:], pb_, sins[:, tsl], ALU.mult)
                    P.tt(dst[:, tsl], t1[k2][:], t2[k2][:], ALU.add)
            for tg in range(4):
                tsl = slice(tg * 512, (tg + 1) * 512)
                P.tt(qd[:, tsl], qrot[:, tsl], qdec[:, h, :], ALU.mult, eng="pool")
            for i in range(NT):
                pv = P.bank(P.rr())[:, 0:256]
                for c in range(8):
                    P.mm(pv, self.XT[:, c, i * 128:(i + 1) * 128], W[:, c, 256:512], start=(c == 0), stop=(c == 7))
                P.copy(vtok[:, i, :], pv[:, 0:128], eng="act")
                P.act(sg[:, i, :], pv[:, 128:256], AF.Silu)
            for g in range(2):
                pt = P.bankb(P.rr())
                for ii in range(8):
                    i = g * 8 + ii
                    P.tr(pt[:, ii * 128:(ii + 1) * 128], krot[:, i * 128:(i + 1) * 128], self.ident[:])
                P.ts(ktok[:, g * 8:(g + 1) * 8, :], pt, kdec[:, h:h + 1], ALU.mult)

        def loop(h):
            q = h % 2
            qrot, krot, qd = qrot_[q], krot_[q], qd_[q]
            vtok, sg, ktok = vtok_[q], sg_[q], ktok_[q]
            state, stateb, scm, yn, yb = state_[q], [stateb_[0][q], stateb_[1][q]], [scm_[0][q], scm_[1][q]], \
                [yn_[0][q], yn_[1][q]], [yb_[0][q], yb_[1][q]]
            st1, mv1, r1 = st1_[q], mv1_[q], r1_[q]
            P.memset(state[:], 0.0)
            P.memset(stateb[0][:], 0.0)
            g128 = float(cst["g128"][h])
            for i in range(NT):
                isl = slice(i * 128, (i + 1) * 128)
                sc, sn = stateb[i % 2], stateb[(i + 1) % 2]
                psc = P.bank(P.rr())[:, 0:128]
                P.mm(psc, krot[:, isl], qrot[:, isl])
                P.tt(scm[i % 2][:], psc, rmask[:, h, :], ALU.mult)
                py = P.bank(P.rr())[:, 0:128]
                P.mm(py, scm[i % 2][:], vtok[:, i, :], start=True, stop=False)
                P.mm(py, qd[:, isl], sc[:], start=False, stop=True)
                if i + 1 < NT:
                    pkv = P.bank(P.rr())[:, 0:128]
                    P.mm(pkv, ktok[:, i, :], vtok[:, i, :])
                    P.stt(state[:], state[:], g128, pkv, ALU.mult, ALU.add)
                    P.copy(sn[:], state[:], eng="act")
                P.op("dve", lambda e, py=py: e.bn_stats(out=st1[:], in_=py), reads=[py], writes=[st1[:]])
                P.op("dve", lambda e: e.bn_aggr(out=mv1[:], in_=st1[:]), reads=[st1[:]], writes=[mv1[:]])
                P.act(r1[:, 0:1], mv1[:, 1:2], AF.Ln, bias=self.eps_t[:, 0:1])
                P.act(r1[:, 1:2], r1[:, 0:1], AF.Exp, scale=-0.5)
                y_ = yn[i % 2]
                P.ts(y_[:], py, mv1[:, 0:1], ALU.subtract, r1[:, 1:2], ALU.mult)
                P.tt(y_[:], y_[:], gng[:, h * 128:(h + 1) * 128], ALU.mult, eng="pool")
                P.tt(yb[i % 2][:], y_[:], sg[:, i, :], ALU.mult, eng="pool")
                pT = P.bankb(P.rr())[:, 0:128]
                P.tr(pT, yb[i % 2][:], self.ident[:])
                P.copy(self.y_retT[:, h, isl], pT, eng="act")

        load_w(0)
        for hp in range((nh + 1) // 2):
            hs = [h for h in (2 * hp, 2 * hp + 1) if h < nh]
            for h in hs:
                if h + 1 < nh:
                    load_w(h + 1)
                    if h + 1 == nh - 1 and self.issue_wr is not None:
                        self.issue_wr()
                        self.issue_wr = None
                front(h)
            P.zipped([lambda h=h: loop(h) for h in hs])
        P.release(m)


def _bc(ap, shape):
    return ap.to_broadcast(list(shape))


def _in_map(inputs, b, kb):
    m = {"x": np.ascontiguousarray(inputs["x"][b])}
    if kb._mem_d is not None:
        m["mem"] = np.ascontiguousarray(inputs["mem"][b])
    for k in kb.prm:
        m[k] = np.ascontiguousarray(inputs[k], dtype=np.float32)
    for k in kb.cst_d:
        m["c_" + k] = _get_consts()[k]
    return m


_NC_CACHE = {}


def kernel(**inputs):
    key = "full"
    if key not in _NC_CACHE:
        kb = K()
        _NC_CACHE[key] = (kb, kb.build())
    kb, nc = _NC_CACHE[key]
    in_maps = [_in_map(inputs, b, kb) for b in range(8)]
    res = run_bass_kernel_spmd(nc, in_maps, core_ids=list(range(8)))
    return np.stack([np.asarray(r["out"], dtype=np.float32) for r in res.results], 0)
```

```python
import numpy as np
import ml_dtypes
from contextlib import ExitStack
import concourse.bass as bass
import concourse.mybir as mybir
from concourse.bass_utils import run_bass_kernel_spmd

F32 = mybir.dt.float32
BF16 = mybir.dt.bfloat16
AF = mybir.ActivationFunctionType
ALU = mybir.AluOpType
AX = mybir.AxisListType

D = 1024
S = 2048
NT = 16
DEPTH = 2
ALPHA = float((2 * DEPTH) ** 0.25)
LN_EPS = 1e-5
N_IN = 5888
NDS = 8
EPOCH = 16000
C0 = float(np.exp(-0.5))


def _prod(xs):
    r = 1
    for v in xs:
        r *= int(v)
    return r


def _dsize(dt):
    return int(mybir.dt.size(dt))


class Prog:
    ENGS = ("pe", "dve", "act", "pool", "sp")

    def __init__(self, nc, es, arena_bytes):
        self.nc = nc
        self.es = es
        self.eng = {"pe": nc.tensor, "dve": nc.vector, "act": nc.scalar,
                    "pool": nc.gpsimd, "sp": nc.sync}
        self.q = {k: [] for k in self.ENGS}
        self.esem = {k: [es.enter_context(nc.semaphore(f"e_{k}_{i}")) for i in range(3)]
                     for k in self.ENGS}
        self.dsem = {k: [es.enter_context(nc.semaphore(f"d_{k}_{i}")) for i in range(NDS)]
                     for k in ("sp", "pool", "act")}
        self.cnt = {k: 0 for k in self.ENGS}
        self.dcnt = {k: [0] * NDS for k in self.dsem}
        self.dnext = {k: 0 for k in self.dsem}
        self.waited = {k: {} for k in self.ENGS}
        self.acc = {}
        self.base = {}
        self.dram = set()
        self.n_instr = 0
        self.out_tokens = []
        slab = es.enter_context(nc.sbuf_tensor("slab", [128, arena_bytes // 4], F32))
        self.arena_lo = int(nc.lookup_mloc("slab").addr)
        self.arena_hi = self.arena_lo + arena_bytes
        self.ptr = self.arena_lo
        self.top = self.arena_hi
        self.uid = 0
        self.x_live = True
        self.bank_ptr = 0
        import os
        self.max_instr = int(os.environ.get("MAXI", str(10 ** 9)))
        self.ps = es.enter_context(nc.psum_tensor("ps", [128, 4096], F32))
        self.base["ps"] = ("PS", 0)
        self.psb = self.ps.bitcast(BF16)

    def alloc(self, name, shape, dtype, top=False):
        nbytes = _prod(shape[1:]) * _dsize(dtype)
        nbytes = (nbytes + 31) // 32 * 32
        if top:
            self.top -= nbytes
            off = self.top
        else:
            off = self.ptr
            self.ptr += nbytes
        lim = self.top if self.x_live else self.arena_hi
        assert self.ptr <= lim, f"SBUF arena overflow at {name}: {self.ptr} > {lim}"
        self.uid += 1
        t = self.nc.alloc_sbuf_tensor_at(f"{name}_{self.uid}", list(shape), dtype, offset=off)
        self.base[t.name] = ("SB", off)
        return t

    def alloc_at(self, name, shape, dtype, off):
        self.uid += 1
        t = self.nc.alloc_sbuf_tensor_at(f"{name}_{self.uid}", list(shape), dtype, offset=off)
        self.base[t.name] = ("SB", off)
        return t

    def mark(self):
        return (self.ptr, self.top)

    def release(self, m):
        self.ptr, self.top = m

    def dram_tensor(self, name, shape, dtype, kind):
        t = self.nc.dram_tensor(name, list(shape), dtype, kind=kind)
        self.dram.add(name)
        return t.ap()

    bank_rng = (0, 8)

    def rr(self, n=1):
        lo, hi = self.bank_rng
        if not hasattr(self, "bank_ptrs"):
            self.bank_ptrs = {}
        p = self.bank_ptrs.get((lo, hi), lo)
        b = (p + n - 1) // n * n
        if b + n > hi:
            b = lo
        self.bank_ptrs[(lo, hi)] = b + n
        return b

    def bank(self, b, n=1):
        return self.ps[:, b * 512:(b + n) * 512]

    def bankb(self, b, n=1):
        return self.psb[:, b * 1024:(b + n) * 1024]

    def _region(self, a):
        t = a.tensor
        name = t.name
        dims = list(a.ap)
        off = int(a.offset)
        sz = _dsize(a.dtype)
        if name in self.dram:
            ext = sum((int(c) - 1) * abs(int(s)) for s, c in dims)
            return ("D:" + name, 0, 1, off * sz, (off + ext + 1) * sz)
        space, base = self.base[name]
        F = _prod(list(t.shape)[1:])
        p0 = off // F
        f0 = off % F
        npart = int(dims[0][1])
        ext = sum((int(c) - 1) * abs(int(s)) for s, c in dims[1:])
        lo = base + f0 * sz
        hi = base + (f0 + ext + 1) * sz
        p1 = p0 + npart
        if space == "PS":
            lo = lo // 2048 * 2048
            hi = (hi + 2047) // 2048 * 2048
            p0 = p0 // 32 * 32
            p1 = (p1 + 31) // 32 * 32
        return (space, p0, p1, lo, hi)

    @staticmethod
    def _overlap(r1, r2):
        return r1[1] < r2[2] and r2[1] < r1[2] and r1[3] < r2[4] and r2[3] < r1[4]

    @staticmethod
    def _covers(r1, r2):
        return r1[1] <= r2[1] and r1[2] >= r2[2] and r1[3] <= r2[3] and r1[4] >= r2[4]

    def _deps(self, reads, writes):
        toks = {}
        rregs = [self._region(a) for a in reads]
        wregs = [self._region(a) for a in writes]
        for r in rregs:
            ps = r[0] == "PS"
            for (reg, key, val, isw) in self.acc.get(r[0], ()):
                if (isw or ps) and self._overlap(reg, r) and toks.get(key, 0) < val:
                    toks[key] = val
        for r in wregs:
            for (reg, key, val, isw) in self.acc.get(r[0], ()):
                if self._overlap(reg, r) and toks.get(key, 0) < val:
                    toks[key] = val
        return toks, rregs, wregs

    def _record(self, rregs, wregs, key, val):
        for r in wregs:
            lst = self.acc.setdefault(r[0], [])
            lst[:] = [e for e in lst if not self._covers(r, e[0])]
            lst.append((r, key, val, True))
        for r in rregs:
            lst = self.acc.setdefault(r[0], [])
            lst[:] = [e for e in lst
                      if not ((not e[3]) and e[1] == key and self._covers(r, e[0]))]
            lst.append((r, key, val, False))

    def _waits_for(self, e, toks):
        ws = []
        for key, val in toks.items():
            if key == ("e", e) and e == "pe":
                continue
            if self.waited[e].get(key, 0) < val:
                self.waited[e][key] = val
                ws.append((key, val))
        return ws

    max_instr = 10 ** 9
    _cap = None
    import os as _os
    zip_gran = int(_os.environ.get("ZGALL", "1"))

    def capture(self, f):
        saved = self._cap
        self._cap = []
        f()
        lst = self._cap
        self._cap = saved
        return lst

    def replay(self, lists):
        idx = [0] * len(lists)
        tot = [max(len(l), 1) for l in lists]
        while True:
            best, bf = -1, 2.0
            for i, l in enumerate(lists):
                if idx[i] < len(l):
                    fr = idx[i] / tot[i]
                    if fr < bf:
                        best, bf = i, fr
            if best < 0:
                break
            for _ in range(self.zip_gran):
                if idx[best] >= len(lists[best]):
                    break
                it = lists[best][idx[best]]
                idx[best] += 1
                if it[0] == "op":
                    self.op(it[1], it[2], it[3], it[4])
                else:
                    self.dma(it[1], it[2], it[3], it[4], **it[5])

    def zipped(self, fns, split=None):
        if self._cap is not None or len(fns) == 1:
            for f in fns:
                f()
            return
        lo0, hi0 = self.bank_rng
        if split is None:
            split = [(hi0 - lo0) // len(fns)] * len(fns)
        lists = []
        lo = lo0
        for k, f in enumerate(fns):
            self.bank_rng = (lo, lo + split[k])
            lo += split[k]
            lists.append(self.capture(f))
        self.bank_rng = (lo0, hi0)
        self.replay(lists)

    def op(self, e, fn, reads=(), writes=()):
        if self._cap is not None:
            self._cap.append(("op", e, fn, tuple(reads), tuple(writes)))
            return
        if self.n_instr >= self.max_instr:
            return
        toks, rregs, wregs = self._deps(reads, writes)
        ws = self._waits_for(e, toks)
        if self.n_instr + 1 == self.max_instr:
            print("LAST INSTR", e, "reads", [(a.tensor.name, a.offset, a.ap) for a in reads], "writes", [(a.tensor.name, a.offset, a.ap) for a in writes], "waits", ws, "cnt", self.cnt)
        self.cnt[e] += 1
        key = ("e", e)
        val = self.cnt[e]
        self.q[e].append((ws, fn, key, val))
        self._record(rregs, wregs, key, val)
        self.n_instr += 1

    def dma(self, qn, out, in_, is_output=False, **kw):
        if self._cap is not None:
            self._cap.append(("dma", qn, out, in_, is_output, kw))
            return
        if self.n_instr >= self.max_instr and not is_output:
            return
        toks, rregs, wregs = self._deps([in_], [out])
        i = self.dnext[qn]
        self.dnext[qn] = (i + 1) % NDS
        key = ("d", qn, i)
        if self.dcnt[qn][i] > 0:
            toks[key] = max(toks.get(key, 0), self.dcnt[qn][i])
        ws = self._waits_for(qn, toks)
        self.dcnt[qn][i] += 16
        val = self.dcnt[qn][i]

        def fn(eng, out=out, in_=in_, kw=kw):
            return eng.dma_start(out=out, in_=in_, **kw)
        self.q[qn].append((ws, fn, key, val))
        self._record(rregs, wregs, key, val)
        self.n_instr += 1
        if is_output:
            self.out_tokens.append((key, val))

    def _sem(self, key, val):
        if key[0] == "e":
            ep = (val - 1) // EPOCH
            return self.esem[key[1]][ep], (val - 1) % EPOCH + 1
        return self.dsem[key[1]][key[2]], val

    def emit(self):
        block = self.es.enter_context(self.nc.Block())
        final = {}
        for key, val in self.out_tokens:
            final[key] = max(final.get(key, 0), val)
        prog = self

        def make(e):
            def body(eng):
                for (ws, fn, key, val) in prog.q[e]:
                    for (k, v) in ws:
                        s, sv = prog._sem(k, v)
                        eng.wait_ge(s, sv)
                    ins = fn(eng)
                    s, sv = prog._sem(key, val)
                    ins.then_inc(s, 16 if key[0] == "d" else 1)
                if e == "sp":
                    for k, v in final.items():
                        s, sv = prog._sem(k, v)
                        eng.wait_ge(s, sv)
            return body
        block.tensor(make("pe"))
        block.vector(make("dve"))
        block.scalar(make("act"))
        block.gpsimd(make("pool"))
        block.sync(make("sp"))

    def mm(self, out, lhsT, rhs, start=True, stop=True):
        self.op("pe", lambda e: e.matmul(out, lhsT=lhsT, rhs=rhs, start=start, stop=stop),
                reads=[lhsT, rhs], writes=[out])

    def tr(self, out, in_, ident):
        self.op("pe", lambda e: e.transpose(out, in_, ident), reads=[in_, ident], writes=[out])

    def act(self, out, in_, func, bias=None, scale=1.0, accum_out=None, eng="act"):
        reads = [in_]
        kw = {}
        if bias is not None:
            kw["bias"] = bias
            if not isinstance(bias, (int, float)):
                reads.append(bias)
        if not isinstance(scale, (int, float)):
            reads.append(scale)
        writes = [out]
        if accum_out is not None:
            kw["accum_out"] = accum_out
            writes.append(accum_out)
        self.op("act", lambda e: e.activation(out=out, in_=in_, func=func, scale=scale, **kw),
                reads=reads, writes=writes)

    def ts(self, out, in0, s1, op0, s2=None, op1=None, eng="dve", accum_out=None):
        reads = [in0]
        for s in (s1, s2):
            if s is not None and not isinstance(s, (int, float)):
                reads.append(s)
        kw = {}
        writes = [out]
        if accum_out is not None:
            kw["accum_out"] = accum_out
            writes.append(accum_out)
        if op1 is None:
            self.op(eng, lambda e: e.tensor_scalar(out=out, in0=in0, scalar1=s1, scalar2=None, op0=op0, **kw),
                    reads=reads, writes=writes)
        else:
            self.op(eng, lambda e: e.tensor_scalar(out=out, in0=in0, scalar1=s1, scalar2=s2,
                                                   op0=op0, op1=op1, **kw),
                    reads=reads, writes=writes)

    def tt(self, out, in0, in1, op, eng="dve"):
        self.op(eng, lambda e: e.tensor_tensor(out=out, in0=in0, in1=in1, op=op),
                reads=[in0, in1], writes=[out])

    def stt(self, out, in0, scalar, in1, op0, op1):
        reads = [in0, in1]
        if not isinstance(scalar, (int, float)):
            reads.append(scalar)
        self.op("dve", lambda e: e.scalar_tensor_tensor(out=out, in0=in0, scalar=scalar, in1=in1,
                                                        op0=op0, op1=op1),
                reads=reads, writes=[out])

    def reduce(self, out, in_, op, axis=AX.X):
        self.op("dve", lambda e: e.tensor_reduce(out=out, in_=in_, axis=axis, op=op), reads=[in_], writes=[out])

    def recip(self, out, in_):
        self.op("dve", lambda e: e.reciprocal(out=out, in_=in_), reads=[in_], writes=[out])

    def copy(self, out, in_, eng="dve"):
        if eng == "act":
            self.act(out, in_, AF.Copy)
        else:
            self.op(eng, lambda e: e.tensor_copy(out=out, in_=in_), reads=[in_], writes=[out])

    def memset(self, out, val, eng="dve"):
        self.op(eng, lambda e: e.memset(out, val), reads=[], writes=[out])


def _consts():
    c = {}
    bf = ml_dtypes.bfloat16
    c["ident"] = np.eye(128, dtype=np.float32).astype(bf)
    blk = np.zeros((128, 128), np.float32)
    blk[:64, :64] = 1.0
    blk[64:, 64:] = 1.0
    c["blkones"] = blk.astype(bf)
    sw = np.zeros((128, 128), np.float32)
    for d in range(128):
        sw[(d + 64) % 128, d] = 1.0
    c["pswap"] = sw.astype(bf)
    half = 64
    inv = (10000.0 ** (-np.arange(half, dtype=np.float32) / half)).astype(np.float32)
    ang = (np.arange(S, dtype=np.float32)[:, None] * inv[None, :]).astype(np.float32)
    cs = np.cos(ang.astype(np.float64)).T
    sn = np.sin(ang.astype(np.float64)).T
    c["cos"] = np.concatenate([cs, cs], 0).astype(np.float32).astype(bf)
    c["sins"] = np.concatenate([-sn, sn], 0).astype(np.float32).astype(bf)
    lg = np.log(1.0 - 2.0 ** (-5.0 - np.arange(4, dtype=np.float64)))
    m = np.arange(128)[:, None]
    cc = np.arange(128)[None, :]
    same = (m // 64) == (cc // 64)
    later = (m // 64) < (cc // 64)
    rmask = np.zeros((4, 128, 128))
    for h in range(4):
        rmask[h] = np.where(same, np.exp(lg[h] * np.abs(cc - m)), 0.0) + np.where(later, np.exp(lg[h] * (cc - m)), 0.0)
    rmask *= 128.0 ** -0.5
    c["rmask"] = np.ascontiguousarray(rmask.transpose(1, 0, 2)).astype(np.float32)
    qdec = np.exp(lg[:, None] * (np.arange(128)[None, :] + 1.0))
    c["qdec"] = np.ascontiguousarray(np.broadcast_to(np.tile(qdec, (1, 4))[None], (128, 4, 512))).astype(np.float32).astype(bf)
    kdec = np.exp(lg[:, None] * (127.0 - np.arange(128)[None, :])) * 128.0 ** -0.5
    c["kdec"] = np.ascontiguousarray(kdec.T).astype(np.float32)
    c["g128"] = np.exp(lg * 128.0)
    s_ = np.arange(64)[:, None]
    t_ = np.arange(64)[None, :]
    strict = (t_ > s_).astype(np.float32)
    incl = (t_ >= s_).astype(np.float32)
    m1 = np.concatenate([strict, incl], 1)
    c["wmask1"] = np.ascontiguousarray(np.broadcast_to(m1[:, None, :], (64, 16, 128))).astype(np.float32).astype(bf)
    low = (t_ < s_).astype(np.float32)
    c["wmask2"] = np.ascontiguousarray(np.broadcast_to(low[:, None, :], (64, 8, 64))).astype(np.float32).astype(bf)
    c["identrep"] = np.ascontiguousarray(np.broadcast_to(np.eye(64, dtype=np.float32)[:, None, :], (64, 8, 64))).astype(bf)
    rst = np.ones((128, 4, 256), np.float32)
    rst[:, :, ::64] = 0.0
    c["rstmask"] = rst
    return c


CONSTS = None


def _get_consts():
    global CONSTS
    if CONSTS is None:
        CONSTS = _consts()
    return CONSTS


_MYDT = {np.dtype(np.float32): F32, np.dtype(ml_dtypes.bfloat16): BF16}

PARAM_SHAPES = {
    'ln_in_g': (1024,), 'ln_in_b': (1024,), 'router_w': (1024, 16), 'router_bias': (16,),
    'w_in': (2, 1024, 5888), 'ret_gn_g': (2, 512), 'rwkv_mu': (2, 1792), 'rwkv_w_up': (2, 64, 512),
    'rwkv_w0': (2, 512), 'rwkv_a_up': (2, 64, 512), 'rwkv_a0': (2, 512), 'rwkv_g_up': (2, 128, 512),
    'rwkv_k_k': (2, 512), 'rwkv_k_a': (2, 512), 'rwkv_r_k': (2, 8, 64), 'rwkv_ln_g': (2, 512),
    'rwkv_ln_b': (2, 512), 'w_ret_up': (2, 512, 1024), 'w_rwkv_up': (2, 512, 1024),
    'w_out': (2, 1024, 1024), 'ln1_g': (2, 1024), 'ln1_b': (2, 1024), 'xa_wq': (2, 1024, 1024),
    'xa_wkv': (2, 1024, 2048), 'xa_wo': (2, 1024, 1024), 'ln2_g': (2, 1024), 'ln2_b': (2, 1024),
    'moe_w_gate': (2, 16, 1024, 512), 'moe_w_up': (2, 16, 1024, 512), 'moe_w_down': (2, 16, 512, 1024),
    'ln3_g': (2, 1024), 'ln3_b': (2, 1024),
}


class K:
    def __init__(self, n_layers=DEPTH, stage="full", taps=()):
        self.n_layers = n_layers
        self.stage = stage
        self.taps = set(taps)
        self.tap_out = {}

    def tap(self, name, ap, shape, dtype):
        if name not in self.taps:
            return
        P = self.P
        d = P.dram_tensor("tap_" + name, shape, dtype, "ExternalOutput")
        P.dma("sp", d, ap, is_output=True)
        self.tap_out[name] = "tap_" + name

    def build(self):
        nc = bass.Bass("TRN2", target_bir_lowering=False)
        self.nc = nc
        self.es = ExitStack()
        es = self.es
        import os
        P = Prog(nc, es, int(os.environ.get('ARENA_KB', '207')) * 1024)
        self.P = P
        self.x_d = P.dram_tensor("x", [S, D], F32, "ExternalInput")
        self._mem_d = None
        self._out_d = None
        self._xs_d = None
        self.prm = {}
        self.lazy = self.stage != "full"
        for k, shp in PARAM_SHAPES.items():
            pass
        self.cst_d = {}
        self.XT = P.alloc("XT", [128, 8, S], BF16)
        self.ident = P.alloc("ident", [128, 128], BF16)
        P.dma("sp", self.ident[:], self.cst("ident"))
        self.blkones = P.alloc("blkones", [128, 128], BF16)
        P.dma("sp", self.blkones[:], self.cst("blkones"))
        self.eps_t = P.alloc("eps", [128, 1], F32)
        P.memset(self.eps_t[:], LN_EPS)
        self.lnG, self.lnB, self.lnGT, self.lnBT, self.xnb = [None], [None], [None], [None], None
        self.X = P.alloc("X", [128, NT, D], F32, top=True)
        self.lnst = [P.alloc(f"lnst{i}", [128, 2, 6], F32) for i in range(4)]
        self.lnmv = [P.alloc(f"lnmv{i}", [128, 2], F32) for i in range(4)]
        self.lnr = [P.alloc(f"lnr{i}", [128, 2], F32) for i in range(4)]
        self.tile_ctr = 0

        self.phase_input()
        if self.stage not in ("ln_in", "ret", "rwkv", "merge", "x1"):
            self.phase_mem()
        if self.stage == "ln_in":
            self.store_X()
            return self.finish()
        for l in range(self.n_layers):
            self.layer(l)
            if self.stage != "full":
                return self.finish()
        self.store_X()
        return self.finish()

    def cst(self, k):
        if k not in self.cst_d:
            v = _get_consts()[k]
            self.cst_d[k] = self.P.dram_tensor("c_" + k, list(v.shape), _MYDT[v.dtype], "ExternalInput")
        return self.cst_d[k]

    @property
    def mem_d(self):
        if self._mem_d is None:
            self._mem_d = self.P.dram_tensor("mem", [256, D], F32, "ExternalInput")
        return self._mem_d

    @property
    def out_d(self):
        if self._out_d is None:
            self._out_d = self.P.dram_tensor("out", [S, D], F32, "ExternalOutput")
        return self._out_d

    @property
    def xs_d(self):
        if self._xs_d is None:
            self._xs_d = self.P.dram_tensor("xspill", [S, D], F32, "Internal")
        return self._xs_d

    def prm_(self, k):
        if k not in self.prm:
            self.prm[k] = self.P.dram_tensor(k, list(PARAM_SHAPES[k]), F32, "ExternalInput")
        return self.prm[k]

    def finish(self):
        self.P.emit()
        return self.nc

    def store_X(self):
        P = self.P
        for i in range(NT):
            P.dma("sp", self.out_d[i * 128:(i + 1) * 128, :], self.X[:, i, :], is_output=True)

    def load_ln(self, g_ap, b_ap):
        P = self.P
        i = 0
        self.lnG[0] = P.alloc("lnG", [128, D], F32)
        self.lnB[0] = P.alloc("lnB", [128, D], F32)
        self.lnGT[0] = P.alloc("lnGT", [128, 8], F32)
        self.lnBT[0] = P.alloc("lnBT", [128, 8], F32)
        self.xnb = [P.alloc(f"xnb{k}", [128, D], BF16) for k in range(4)]
        P.dma("sp", self.lnG[i][:], g_ap.partition_broadcast(128))
        P.dma("sp", self.lnB[i][:], b_ap.partition_broadcast(128))
        P.dma("sp", self.lnGT[i][:], g_ap.rearrange("(c p) -> p c", p=128), allow_slow_non_contiguous=True)
        P.dma("sp", self.lnBT[i][:], b_ap.rearrange("(c p) -> p c", p=128), allow_slow_non_contiguous=True)
        return i

    def ln_tile(self, i, li, part=None, xb=None):
        P = self.P
        Xt = self.X[:, i, :]
        st, mv, r = self.lnst[i % 4], self.lnmv[i % 4], self.lnr[i % 4]
        if xb is None:
            xb = self.xnb[i % 4]
        if part == "b":
            return self.ln_tile_b(i, li, xb)
        for hlf in range(2):
            P.op("dve", lambda e, hlf=hlf: e.bn_stats(out=st[:, hlf, :], in_=self.X[:, i, hlf * 512:(hlf + 1) * 512]),
                 reads=[self.X[:, i, hlf * 512:(hlf + 1) * 512]], writes=[st[:, hlf, :]])
        P.op("dve", lambda e: e.bn_aggr(out=mv[:], in_=st[:]), reads=[st[:]], writes=[mv[:]])
        import os
        dbg = int(os.environ.get("DBG_LN", "9"))
        if dbg < 2:
            return
        P.act(r[:, 0:1], mv[:, 1:2], AF.Ln, bias=self.eps_t[:, 0:1])
        P.act(r[:, 1:2], r[:, 0:1], AF.Exp, scale=-0.5)
        if dbg < 3:
            return
        P.ts(Xt, Xt, mv[:, 0:1], ALU.subtract, r[:, 1:2], ALU.mult)
        if dbg < 4:
            return
        self.tile_ctr += 1
        P.copy(xb[:], Xt, eng="act")
        if dbg < 5:
            return
        P.tt(Xt, Xt, self.lnG[li][:], ALU.mult, eng="pool")
        P.tt(Xt, Xt, self.lnB[li][:], ALU.add, eng="pool")
        if dbg < 6 or part == "a":
            return
        self.ln_tile_b(i, li, xb)

    def ln_tile_b(self, i, li, xb):
        P = self.P
        dbg = 9
        pb = P.rr(2) // 2
        pts = [P.psb[:, (2 * pb + c % 2) * 1024 + (c // 2) * 128:(2 * pb + c % 2) * 1024 + (c // 2) * 128 + 128]
               for c in range(8)]
        for c in range(8):
            P.tr(pts[c], xb[:, c * 128:(c + 1) * 128], self.ident[:])
        if dbg < 7:
            return
        for c in range(8):
            dst = self.XT[:, c, i * 128:(i + 1) * 128]
            src = pts[c]
            if (c % 2 == 0 or dbg == 7) and dbg != 8:
                P.ts(dst, src, self.lnGT[li][:, c:c + 1], ALU.mult, self.lnBT[li][:, c:c + 1], ALU.add)
            else:
                P.act(dst, src, AF.Identity, bias=self.lnBT[li][:, c:c + 1], scale=self.lnGT[li][:, c:c + 1])

    def phase_input(self):
        P = self.P
        m = P.mark()
        li = self.load_ln(self.prm_("ln_in_g"), self.prm_("ln_in_b"))
        def one(i):
            P.dma("sp", self.X[:, i, :], self.x_d[i * 128:(i + 1) * 128, :])
            self.ln_tile(i, li)
        for i in range(0, NT, 4):
            P.zipped([lambda i=i, k=k: one(i + k) for k in range(4)])
        P.release(m)
        self.tap("xt0", self.XT[:, :, 0:256], [128, 8, 256], BF16)

    def layer(self, l):
        P = self.P
        def spill():
            for i in range(NT):
                P.dma("sp", self.xs_d[i * 128:(i + 1) * 128, :], self.X[:, i, :])
        self.spill = spill
        P.x_live = False
        m0 = P.mark()
        xbase = P.base[self.X.name][1]
        ybase = xbase + 64 * 1024 - 2 * 4 * S * 2
        self.y_retT = P.alloc_at("y_retT", [128, 4, S], BF16, ybase)
        self.y_rwkvT = P.alloc_at("y_rwkvT", [128, 4, S], BF16, ybase + 4 * S * 2)
        P.arena_hi_save = P.arena_hi
        P.arena_hi = ybase
        if "ret1h" in self.taps:
            P.memset(self.y_retT[:], 0.0)
        if "rwkv_short" in self.taps:
            P.memset(self.y_rwkvT[:], 0.0)
        self.Wr = P.alloc("Wr", [128, 8, 1792], BF16)

        def issue_wr():
            w_in = self.prm_("w_in")
            for j in range(7):
                src = w_in[l, :, 2048 + j * 256:2048 + (j + 1) * 256].rearrange("(c p) n -> p c n", p=128)
                P.dma("pool", self.Wr[:, :, j * 256:(j + 1) * 256], src)
        self.issue_wr = issue_wr
        self.phase_ret(l)
        if self.issue_wr is not None:
            self.issue_wr()
            self.issue_wr = None
        self.tap("yretT", self.y_retT[:], [128, 4, S], BF16)
        if self.stage == "ret":
            return
        self.wru = P.alloc("wru", [128, 4, D], BF16)
        self.wwu = P.alloc("wwu", [128, 4, D], BF16)

        def issue_wu():
            P.dma("pool", self.wru[:], self.prm_("w_ret_up")[l].rearrange("(c p) n -> p c n", p=128))
            P.dma("pool", self.wwu[:], self.prm_("w_rwkv_up")[l].rearrange("(c p) n -> p c n", p=128))
        self.issue_wu = issue_wu
        self.phase_rwkv(l)
        self.tap("yrwkvT", self.y_rwkvT[:], [128, 4, S], BF16)
        if self.stage == "rwkv":
            return
        self.mergedT = P.alloc("mergedT", [128, 8, S], BF16)
        self.wo_buf = P.alloc("w_out", [128, 8, D], BF16)
        self.phase_merge(l)
        self.tap("mergedT", self.mergedT[:, :, 0:512], [128, 8, 512], BF16)
        if self.stage == "merge":
            return
        P.arena_hi = P.arena_hi_save
        P.x_live = True
        ptr_save = P.ptr
        P.ptr = P.base[self.Wr.name][1]
        self.phase_out(l)
        assert P.ptr <= P.base[self.mergedT.name][1]
        P.ptr = ptr_save
        P.release(m0)
        self.tap(f"x1_{l}", self.X[:], [128, NT, D], F32)
        if self.stage == "x1":
            return
        self.phase_xattn(l)
        self.tap(f"x2_{l}", self.X[:], [128, NT, D], F32)
        if self.stage == "x2":
            return
        self.phase_moe(l)
        self.tap(f"x3_{l}", self.X[:], [128, NT, D], F32)

    def phase_merge(self, l):
        P = self.P
        m = P.mark()
        w_in = self.prm_("w_in")
        wru, wwu = self.wru, self.wwu
        Wg = [P.alloc(f"Wg{i}", [128, 8, 256], BF16) for i in range(2)]
        sgr = [P.alloc(f"sgr{i}", [128, 512], F32) for i in range(2)]
        sgw = [P.alloc(f"sgw{i}", [128, 512], F32) for i in range(2)]

        def load_g(cc):
            W = Wg[cc % 2]
            for j in range(2):
                c0 = 3840 + j * 1024 + cc * 128
                P.dma("pool", W[:, :, j * 128:(j + 1) * 128], w_in[l, :, c0:c0 + 128].rearrange("(c p) n -> p c n", p=128))
        load_g(0)
        k = 0
        for cc in range(8):
            if cc + 1 < 8:
                load_g(cc + 1)
            if cc == 3:
                for j in range(2):
                    P.dma("pool", self.wo_buf[:, :, j * 512:(j + 1) * 512],
                          self.prm_("w_out")[l, :, j * 512:(j + 1) * 512].rearrange("(c p) n -> p c n", p=128))
            W = Wg[cc % 2]
            csl = slice(cc * 128, (cc + 1) * 128)
            for tg in range(4):
                tsl = slice(tg * 512, (tg + 1) * 512)
                pgr = P.bank(P.rr())
                for c in range(8):
                    P.mm(pgr, W[:, c, 0:128], self.XT[:, c, tsl], start=(c == 0), stop=(c == 7))
                pgw = P.bank(P.rr())
                for c in range(8):
                    P.mm(pgw, W[:, c, 128:256], self.XT[:, c, tsl], start=(c == 0), stop=(c == 7))
                pur = P.bank(P.rr())
                for c in range(4):
                    P.mm(pur, wru[:, c, csl], self.y_retT[:, c, tsl], start=(c == 0), stop=(c == 3))
                puw = P.bank(P.rr())
                for c in range(4):
                    P.mm(puw, wwu[:, c, csl], self.y_rwkvT[:, c, tsl], start=(c == 0), stop=(c == 3))
                a, b = sgr[k % 2], sgw[k % 2]
                k += 1
                P.act(a[:], pgr, AF.Sigmoid)
                P.act(b[:], pgw, AF.Sigmoid)
                P.tt(a[:], a[:], pur, ALU.mult)
                P.tt(b[:], b[:], puw, ALU.mult)
                P.tt(self.mergedT[:, cc, tsl], a[:], b[:], ALU.add)
        P.release(m)

    def phase_out(self, l):
        P = self.P
        m = P.mark()
        wo = self.wo_buf
        xin = [P.alloc(f"xin{i}", [128, D], F32) for i in range(4)]
        li = self.load_ln(self.prm_("ln1_g")[l], self.prm_("ln1_b")[l])
        assert P.ptr <= P.base[self.mergedT.name][1], "phase_out scratch runs into mergedT"
        def one(i):
            isl = slice(i * 128, (i + 1) * 128)
            P.dma("sp", xin[i % 4][:], self.xs_d[isl, :])
            b = P.rr(2)
            ph = P.bank(b, 2)
            for hf in range(2):
                for c in range(8):
                    P.mm(ph[:, hf * 512:(hf + 1) * 512], self.mergedT[:, c, isl], wo[:, c, hf * 512:(hf + 1) * 512],
                         start=(c == 0), stop=(c == 7))
            P.stt(self.X[:, i, :], xin[i % 4][:], ALPHA, ph, ALU.mult, ALU.add)
            self.ln_tile(i, li)
        for i in range(0, NT, 4):
            P.zipped([lambda i=i, k=k: one(i + k) for k in range(4)])
        P.release(m)

    def phase_mem(self):
        P = self.P
        self.memT = P.alloc("memT", [128, 8, 256], BF16)
        m = P.mark()
        mt = P.alloc("memtmp", [128, D], F32)
        mb = P.alloc("memb", [128, D], BF16)
        for t in range(2):
            P.dma("sp", mt[:], self.mem_d[t * 128:(t + 1) * 128, :])
            P.copy(mb[:], mt[:], eng="act")
            pt = P.bankb(P.rr())
            for c in range(8):
                P.tr(pt[:, c * 128:(c + 1) * 128], mb[:, c * 128:(c + 1) * 128], self.ident[:])
            P.copy(self.memT[:, :, t * 128:(t + 1) * 128], pt.rearrange("p (c k) -> p c k", k=128))
        P.release(m)

    def phase_xattn(self, l):
        P = self.P
        m = P.mark()
        kT = P.alloc("kT", [128, 8, 256], BF16)
        vtok = P.alloc("vtokx", [128, 2, D], BF16)
        qT = P.alloc("qTx", [128, 8, S], BF16)
        wkv = self.prm_("xa_wkv")
        wA = P.alloc("wxa", [128, 8, D], BF16)
        wB = P.alloc("wxb", [128, 8, D], BF16)
        m1 = P.mark()
        wk = wA
        for j in range(2):
            P.dma("pool", wk[:, :, j * 512:(j + 1) * 512],
                  wkv[l, :, j * 512:(j + 1) * 512].rearrange("(c p) n -> p c n", p=128))
        wv = wB
        for j in range(2):
            P.dma("pool", wv[:, :, j * 512:(j + 1) * 512],
                  wkv[l, :, D + j * 512:D + (j + 1) * 512].rearrange("(c p) n -> p c n", p=128))
        for cc in range(8):
            pk = P.bank(P.rr())[:, 0:256]
            for c in range(8):
                P.mm(pk, wk[:, c, cc * 128:(cc + 1) * 128], self.memT[:, c, :], start=(c == 0), stop=(c == 7))
            P.copy(kT[:, cc, :], pk, eng="act")
        P.release(m1)
        wq = wA
        for j in range(2):
            P.dma("pool", wq[:, :, j * 512:(j + 1) * 512],
                  self.prm_("xa_wq")[l, :, j * 512:(j + 1) * 512].rearrange("(c p) n -> p c n", p=128))
        for mt in range(2):
            for hf in range(2):
                pv = P.bank(P.rr())
                for c in range(8):
                    P.mm(pv, self.memT[:, c, mt * 128:(mt + 1) * 128], wv[:, c, hf * 512:(hf + 1) * 512],
                         start=(c == 0), stop=(c == 7))
                P.copy(vtok[:, mt, hf * 512:(hf + 1) * 512], pv, eng="act")
        P.release(m1)
        wo = wB
        for j in range(2):
            P.dma("pool", wo[:, :, j * 512:(j + 1) * 512],
                  self.prm_("xa_wo")[l, :, j * 512:(j + 1) * 512].rearrange("(c p) n -> p c n", p=128))
        k = 0
        for cc in range(8):
            for tg in range(4):
                tsl = slice(tg * 512, (tg + 1) * 512)
                pq = P.bank(P.rr())
                for c in range(8):
                    P.mm(pq, wq[:, c, cc * 128:(cc + 1) * 128], self.XT[:, c, tsl], start=(c == 0), stop=(c == 7))
                if k % 2 == 0:
                    P.act(qT[:, cc, tsl], pq, AF.Copy, scale=1.0 / 16.0)
                else:
                    P.ts(qT[:, cc, tsl], pq, 1.0 / 16.0, ALU.mult)
                k += 1
        P.release(m1)
        li = self.load_ln(self.prm_("ln2_g")[l], self.prm_("ln2_b")[l])
        P.ts(self.lnG[li][:], self.lnG[li][:], ALPHA, ALU.mult, eng="pool")
        P.ts(self.lnB[li][:], self.lnB[li][:], ALPHA, ALU.mult, eng="pool")
        mxs = [P.alloc(f"mx{i}", [128, 4], F32) for i in range(2)]
        nmxs = [P.alloc(f"nmx{i}", [128, 4], F32) for i in range(2)]
        ssums = [P.alloc(f"ssum{i}", [128, 4], F32) for i in range(2)]
        rss = [P.alloc(f"rsx{i}", [128, 4], F32) for i in range(2)]
        pe_ = [P.alloc(f"pe{i}", [128, 4, 256], BF16) for i in range(2)]
        pT = [P.alloc(f"pT{i}", [128, 8, 128], BF16) for i in range(2)]
        oT = [P.alloc(f"oT{i}", [128, 8, 128], BF16) for i in range(2)]

        def one(i):
            mx, nmx, ssum, rs = mxs[i % 2], nmxs[i % 2], ssums[i % 2], rss[i % 2]
            isl = slice(i * 128, (i + 1) * 128)
            b = P.rr(2)
            ps = P.bank(b, 2)
            for h in range(4):
                for dc in range(2):
                    P.mm(ps[:, h * 256:(h + 1) * 256], qT[:, 2 * h + dc, isl], kT[:, 2 * h + dc, :],
                         start=(dc == 0), stop=(dc == 1))
            P.reduce(mx[:], ps.rearrange("p (h m) -> p h m", h=4), ALU.max)
            P.ts(nmx[:], mx[:], -1.0, ALU.mult)
            pe = pe_[i % 2]
            for h in range(4):
                P.act(pe[:, h, :], ps[:, h * 256:(h + 1) * 256], AF.Exp, bias=nmx[:, h:h + 1],
                      accum_out=ssum[:, h:h + 1])
            P.recip(rs[:], ssum[:])
            P.tt(pe[:], pe[:], _bc(rs[:].unsqueeze(2), [128, 4, 256]), ALU.mult)
            ppT = P.bankb(P.rr())
            for h in range(4):
                for mt in range(2):
                    j = h * 2 + mt
                    P.tr(ppT[:, j * 128:(j + 1) * 128], pe[:, h, mt * 128:(mt + 1) * 128], self.ident[:])
            P.copy(pT[i % 2][:].rearrange("p a b -> p (a b)"), ppT, eng="act")
            b = P.rr(2)
            po = P.bank(b, 2)
            for h in range(4):
                for ec in range(2):
                    j = h * 2 + ec
                    for mt in range(2):
                        P.mm(po[:, j * 128:(j + 1) * 128], vtok[:, mt, h * 256 + ec * 128:h * 256 + (ec + 1) * 128],
                             pT[i % 2][:, h * 2 + mt, :], start=(mt == 0), stop=(mt == 1))
            P.copy(oT[i % 2][:].rearrange("p a b -> p (a b)"), po)
            b = P.rr(2)
            ph = P.bank(b, 2)
            for hf in range(2):
                for c in range(8):
                    P.mm(ph[:, hf * 512:(hf + 1) * 512], oT[i % 2][:, c, :], wo[:, c, hf * 512:(hf + 1) * 512],
                         start=(c == 0), stop=(c == 7))
            P.stt(self.X[:, i, :], self.X[:, i, :], ALPHA, ph, ALU.mult, ALU.add)
            self.ln_tile(i, li)
        for i in range(0, NT, 2):
            P.zipped([lambda i=i: one(i), lambda i=i: one(i + 1)])
        P.release(m)

    def phase_moe(self, l):
        P = self.P
        m = P.mark()
        XT = self.XT
        wg = [P.alloc(f"wg{i}", [128, 8, 512], BF16) for i in range(2)]
        wu = [P.alloc(f"wu{i}", [128, 8, 512], BF16) for i in range(2)]
        wd = [P.alloc(f"wd{i}", [128, 4, D], BF16) for i in range(2)]
        hT = [P.alloc(f"hT{i}", [128, 4, 512], BF16) for i in range(2)]
        sgl = [P.alloc(f"sgl{i}", [128, 512], F32) for i in range(2)]
        mg, mu_, md = self.prm_("moe_w_gate"), self.prm_("moe_w_up"), self.prm_("moe_w_down")

        def load_e(e):
            P.dma("pool", wg[e % 2][:], mg[l, e].rearrange("(c p) n -> p c n", p=128))
            P.dma("pool", wu[e % 2][:], mu_[l, e].rearrange("(c p) n -> p c n", p=128))
            for j in range(2):
                P.dma("pool", wd[e % 2][:, :, j * 512:(j + 1) * 512],
                      md[l, e, :, j * 512:(j + 1) * 512].rearrange("(c p) n -> p c n", p=128))
        load_e(0)
        rw = P.alloc("rw", [128, 8, 16], BF16)
        P.dma("pool", rw[:], self.prm_("router_w").rearrange("(c p) e -> p c e", p=128))
        rb = P.alloc("rb", [128, 16], F32)
        P.dma("sp", rb[:], self.prm_("router_bias").partition_broadcast(128))

        def R(name, n=256):
            return P.alloc(name, [128, n], F32)
        aff, ch, t_, mc, sel1, sel2, w_ = R("aff"), R("ch"), R("t_"), R("mc"), R("sel1"), R("sel2"), R("w_")
        comb = R("comb")
        ps6 = R("ps6", 64 * 6)
        gs, ing, pen = R("gs", 64), R("ing", 64), R("pen", 64)
        gmax, m1_, m2_, wsum, rws = R("gmax", 16), R("m1", 16), R("m2", 16), R("wsum", 16), R("rws", 16)
        pl = P.bank(P.rr())
        for i in range(NT):
            for c in range(8):
                P.mm(pl[:, i * 16:(i + 1) * 16], XT[:, c, i * 128:(i + 1) * 128], rw[:, c, :], start=(c == 0), stop=(c == 7))
        P.act(aff[:], pl[:, 0:256], AF.Sigmoid)
        v3 = lambda t: t[:].rearrange("p (a e) -> p a e", e=16)
        g3 = lambda t: t[:].rearrange("p (a e) -> p a e", e=4)
        P.tt(v3(ch), v3(aff), _bc(rb[:].unsqueeze(1), [128, 16, 16]), ALU.add)
        c4 = g3(ch)
        p6 = ps6[:].rearrange("p (a k) -> p a k", k=6)
        P.tt(p6[:, :, 0:3], _bc(c4[:, :, 0:1], [128, 64, 3]), c4[:, :, 1:4], ALU.add)
        P.tt(p6[:, :, 3:5], _bc(c4[:, :, 1:2], [128, 64, 2]), c4[:, :, 2:4], ALU.add)
        P.tt(p6[:, :, 5:6], c4[:, :, 2:3], c4[:, :, 3:4], ALU.add)
        P.reduce(gs[:], p6, ALU.max)
        gs3 = gs[:].rearrange("p (a g) -> p a g", g=4)
        P.reduce(gmax[:], gs3, ALU.max)
        P.tt(ing[:].rearrange("p (a g) -> p a g", g=4), gs3, _bc(gmax[:].unsqueeze(2), [128, 16, 4]), ALU.is_equal)
        P.ts(pen[:], ing[:], 1.0, ALU.subtract, 1e30, ALU.mult)
        P.tt(g3(t_), c4, _bc(ing[:].unsqueeze(2), [128, 64, 4]), ALU.mult)
        P.tt(g3(mc), g3(t_), _bc(pen[:].unsqueeze(2), [128, 64, 4]), ALU.add)
        P.reduce(m1_[:], v3(mc), ALU.max)
        P.tt(v3(sel1), v3(mc), _bc(m1_[:].unsqueeze(2), [128, 16, 16]), ALU.is_equal)
        P.stt(t_[:], sel1[:], -1e30, mc[:], ALU.mult, ALU.add)
        P.reduce(m2_[:], v3(t_), ALU.max)
        P.tt(v3(sel2), v3(t_), _bc(m2_[:].unsqueeze(2), [128, 16, 16]), ALU.is_equal)
        P.tt(sel1[:], sel1[:], sel2[:], ALU.add)
        P.tt(w_[:], aff[:], sel1[:], ALU.mult)
        P.reduce(wsum[:], v3(w_), ALU.add)
        P.recip(rws[:], wsum[:])
        P.tt(v3(comb), v3(w_), _bc(rws[:].unsqueeze(2), [128, 16, 16]), ALU.mult)
        self.tap(f"comb{l}", comb[:], [128, 256], F32)
        ne = 16
        k = 0
        li = self.load_ln(self.prm_("ln3_g")[l], self.prm_("ln3_b")[l])
        xnb4 = [P.alloc(f"xnb4_{i}", [128, D], BF16) for i in range(4)]
        pending = []
        for e in range(ne):
            if e + 1 < ne:
                load_e(e + 1)
            g_, u_, d_ = wg[e % 2], wu[e % 2], wd[e % 2]
            for tg in range(4):
                tsl = slice(tg * 512, (tg + 1) * 512)
                h_ = hT[(e * 4 + tg) % 2]
                for fc in range(4):
                    pg = P.bank(P.rr())
                    for c in range(8):
                        P.mm(pg, g_[:, c, fc * 128:(fc + 1) * 128], XT[:, c, tsl], start=(c == 0), stop=(c == 7))
                    pu = P.bank(P.rr())
                    for c in range(8):
                        P.mm(pu, u_[:, c, fc * 128:(fc + 1) * 128], XT[:, c, tsl], start=(c == 0), stop=(c == 7))
                    sg_ = sgl[k % 2]
                    k += 1
                    P.act(sg_[:], pg, AF.Silu)
                    P.tt(h_[:, fc, :], sg_[:], pu, ALU.mult)
                for i in pending:
                    self.ln_tile(i, li, part="b", xb=xnb4[i % 4])
                pending = []
                for ti in range(4):
                    i = tg * 4 + ti
                    b = P.rr(2)
                    pd = P.bank(b, 2)
                    for hf in range(2):
                        for fc in range(4):
                            P.mm(pd[:, hf * 512:(hf + 1) * 512], h_[:, fc, ti * 128:(ti + 1) * 128],
                                 d_[:, fc, hf * 512:(hf + 1) * 512], start=(fc == 0), stop=(fc == 3))
                    P.stt(self.X[:, i, :], pd, comb[:, i * 16 + e:i * 16 + e + 1], self.X[:, i, :], ALU.mult, ALU.add)
                if e == ne - 1:
                    for ti in range(4):
                        i = tg * 4 + ti
                        self.ln_tile(i, li, part="a", xb=xnb4[i % 4])
                        pending.append(i)
        for i in pending:
            self.ln_tile(i, li, part="b", xb=xnb4[i % 4])
        P.release(m)

    def phase_rwkv(self, l):
        P = self.P
        m = P.mark()
        import os
        dbgrw = int(os.environ.get("DBG_RW", "99"))
        TB = 128
        NCH = TB // 64
        NB = S // TB
        if "rwkv_short" in self.taps:
            NB = 2
        w_in = self.prm_("w_in")
        Wr = self.Wr
        waup = P.alloc("waup", [128, 512], BF16)
        P.dma("pool", waup[0:64, :], self.prm_("rwkv_w_up")[l])
        P.dma("pool", waup[64:128, :], self.prm_("rwkv_a_up")[l])
        gup = P.alloc("gup", [128, 512], BF16)
        P.dma("pool", gup[:], self.prm_("rwkv_g_up")[l])
        self.issue_wu()
        cols = P.alloc("cols", [128, 7, 4], F32)
        names = ["rwkv_w0", "rwkv_a0", "rwkv_k_k", "rwkv_k_a", None, "rwkv_ln_g", "rwkv_ln_b"]
        for idx, nm in enumerate(names):
            if nm is None:
                src = self.prm_("rwkv_r_k")[l].rearrange("h d -> (h d)")
            else:
                src = self.prm_(nm)[l]
            P.dma("sp", cols[:, idx, :], src.rearrange("(a p) -> p a", p=128), allow_slow_non_contiguous=True)
        muT = P.alloc("muT", [128, 14], F32)
        P.dma("sp", muT[:], self.prm_("rwkv_mu")[l].rearrange("(a p) -> p a", p=128), allow_slow_non_contiguous=True)
        wmask1 = P.alloc("wmask1", [64, 16, 128], BF16)
        P.dma("sp", wmask1[:], self.cst("wmask1"))
        wmask2 = P.alloc("wmask2", [64, 8, 64], BF16)
        P.dma("sp", wmask2[:], self.cst("wmask2"))
        identrep = P.alloc("identrep", [64, 8, 64], BF16)
        P.dma("sp", identrep[:], self.cst("identrep"))
        rstm = P.alloc("rstm", [128, 4, TB], F32)
        P.dma("sp", rstm[:], self.cst("rstmask")[:, :, 0:TB])
        epsg = P.alloc("epsg", [128, 1], F32)
        P.memset(epsg[:], 64e-5)

        def A4(name, dt=F32):
            return P.alloc(name, [128, 4, TB], dt)

        def D2(name, dt=F32):
            a = A4(name, dt)
            return [a, a]
        praw = [P.alloc(f"praw{i}", [128, TB + 1], F32) for i in range(2)]
        dtmp = [P.alloc(f"dtmp{i}", [128, TB], F32) for i in range(2)]
        r_, k_, v_ = A4("r"), A4("k"), A4("v")
        dwda = P.alloc("dwda", [128, TB], F32)
        dg = P.alloc("dg", [128, TB], F32)
        th = P.alloc("th", [128, TB], BF16)
        sgd = P.alloc("sgd", [128, TB], BF16)
        sig, Lsig, Lm, emL = A4("sig"), A4("Lsig"), A4("Lm"), A4("emL")
        a_, kk, kkn, k2, tmp, rn = A4("a"), A4("kk"), A4("kkn"), A4("k2"), A4("tmp"), A4("rn")
        sq = A4("sq", BF16)
        rkb = A4("rkb", BF16)
        eL_ = D2("eL")
        g__ = D2("g", BF16)
        vb_ = D2("vb", BF16)
        bonus_ = [sig, sig]
        AR_ = [P.alloc("AR", [128, 4, NCH, 2, 64], BF16)] * 2
        BK_ = [P.alloc("BK", [128, 4, NCH, 2, 64], BF16)] * 2
        BKe_ = [P.alloc("BKe", [128, 4, NCH, 2, 64], BF16)] * 2
        YT = Lsig
        YTb = A4("YTb", BF16)
        sqp = A4("sqp", BF16)
        rnp = A4("rnp")
        cen = YT
        Vtok = [P.alloc(f"Vtok{i}", [64, 512], BF16) for i in range(2)]
        Btok = [P.alloc(f"Btok{i}", [64, 1024], BF16) for i in range(2)]
        AM = [P.alloc(f"AM{i}", [64, 8, 2, 128], BF16) for i in range(2)]
        Qb = [P.alloc(f"Qb{i}", [64, NCH * 8, 64], BF16) for i in range(2)]
        Pb = [P.alloc(f"Pb{i}", [64, NCH * 8, 64], BF16) for i in range(2)]
        Xb = [P.alloc(f"Xb{i}", [64, NCH * 8, 64], BF16) for i in range(2)]
        G2 = P.alloc("G2", [64, 512], F32)
        Y2 = P.alloc("Y2", [64, 512], F32)
        Gs = P.alloc("Gs", [64, 512], BF16)
        Us = P.alloc("Us", [64, 512], BF16)
        Yt = P.alloc("Yt", [64, 512], F32)
        Ytb = P.alloc("Ytb", [64, 512], BF16)
        T = P.alloc("T", [128, 4, 64], F32)
        Tb = [P.alloc(f"Tb{i}", [128, 4, 64], BF16) for i in range(2)]
        P.memset(T[:], 0.0)
        P.memset(Tb[0][:], 0.0)
        XT = self.XT
        blk = self.blkones
        ident = self.ident

        def f2(t):
            return t[:].rearrange("p a b -> p (a b)")

        def v4(t):
            return t[:].rearrange("p a (n t) -> p a n t", t=64)

        def front(tb, part):
            q = tb % 2
            eL, g_, vb, bonus, AR, BK, BKe = eL_[q], g__[q], vb_[q], bonus_[q], AR_[q], BK_[q], BKe_[q]
            t0 = tb * TB
            lo = 0 if tb > 0 else 1
            for j in range(14 if part == 0 else 0):
                pa = P.bank(P.rr())
                for c in range(8):
                    P.mm(pa[:, lo:TB + 1], Wr[:, c, j * 128:(j + 1) * 128], XT[:, c, t0 - 1 + lo:t0 + TB],
                         start=(c == 0), stop=(c == 7))
                pr = praw[j % 2]
                P.copy(pr[:, lo:TB + 1], pa[:, lo:TB + 1], eng="act")
                if tb == 0:
                    P.memset(pr[:, 0:1], 0.0)
                d = dtmp[j % 2]
                P.tt(d[:], pr[:, 0:TB], pa[:, 1:TB + 1], ALU.subtract)
                if j < 4:
                    dst = r_[:, j, :]
                elif j < 8:
                    dst = k_[:, j - 4, :]
                elif j < 12:
                    dst = v_[:, j - 8, :]
                elif j == 12:
                    dst = dwda[:]
                else:
                    dst = dg[:]
                P.stt(dst, d[:], muT[:, j:j + 1], pa[:, 1:TB + 1], ALU.mult, ALU.add)
            if part == 0:
                return
            P.act(th[0:64, :], dwda[0:64, :], AF.Tanh)
            P.copy(th[64:128, :], dwda[64:128, :], eng="act")
            P.act(sgd[:], dg[:], AF.Sigmoid)
            pz = P.bank(P.rr())
            for p in range(4):
                P.mm(pz[:, p * TB:(p + 1) * TB], waup[0:64, p * 128:(p + 1) * 128], th[0:64, :])
            for p in range(4):
                P.act(sig[:, p, :], pz[:, p * TB:(p + 1) * TB], AF.Sigmoid, bias=cols[:, 0, p:p + 1])
            pa_ = P.bank(P.rr())
            for p in range(4):
                P.mm(pa_[:, p * TB:(p + 1) * TB], waup[64:128, p * 128:(p + 1) * 128], th[64:128, :])
            for p in range(4):
                P.act(a_[:, p, :], pa_[:, p * TB:(p + 1) * TB], AF.Sigmoid, bias=cols[:, 1, p:p + 1])
            pg = P.bank(P.rr())
            for p in range(4):
                P.mm(pg[:, p * TB:(p + 1) * TB], gup[:, p * 128:(p + 1) * 128], sgd[:])
            P.copy(f2(g_), pg[:, 0:4 * TB], eng="act")
            P.op("dve", lambda e: e.tensor_tensor_scan(out=f2(Lsig), data0=f2(rstm), data1=f2(sig), initial=0.0,
                                                       op0=ALU.mult, op1=ALU.add),
                 reads=[f2(rstm), f2(sig)], writes=[f2(Lsig)])
            P.tt(f2(Lm), f2(Lsig), f2(sig), ALU.subtract)
            P.act(f2(eL), f2(Lsig), AF.Exp, scale=-C0)
            P.act(f2(emL), f2(Lsig), AF.Exp, scale=C0)
            P.act(f2(Lm), f2(Lm), AF.Exp, scale=-C0)
            P.tt(kk[:], k_[:], _bc(cols[:, 2, :].unsqueeze(2), [128, 4, TB]), ALU.mult)
            P.tt(f2(sq), f2(kk), f2(kk), ALU.mult)
            pss = P.bank(P.rr())
            for p in range(4):
                P.mm(pss[:, p * TB:(p + 1) * TB], blk[:], sq[:, p, :])
            P.ts(f2(rn), pss[:, 0:4 * TB], 1e-19, ALU.max)
            P.act(f2(rn), f2(rn), AF.Ln)
            P.act(f2(rn), f2(rn), AF.Exp, scale=-0.5)
            P.tt(f2(kkn), f2(kk), f2(rn), ALU.mult)
            P.ts(f2(kk), f2(kkn), -1.0, ALU.mult)
            P.ts(f2(tmp), f2(a_), 1.0, ALU.subtract)
            P.tt(tmp[:], tmp[:], _bc(cols[:, 3, :].unsqueeze(2), [128, 4, TB]), ALU.mult)
            P.stt(f2(k2), f2(tmp), 1.0, f2(k_), ALU.add, ALU.mult)
            P.tt(AR[:, :, :, 0, :], v4(kk), v4(Lm), ALU.mult)
            P.tt(AR[:, :, :, 1, :], v4(r_), v4(eL), ALU.mult)
            P.tt(f2(tmp), f2(kkn), f2(a_), ALU.mult)
            P.tt(BK[:, :, :, 0, :], v4(tmp), v4(emL), ALU.mult)
            P.tt(BK[:, :, :, 1, :], v4(k2), v4(emL), ALU.mult)
            for p in range(4):
                for n in range(NCH):
                    P.ts(BKe[:, p, n, :, :], BK[:, p, n, :, :], eL[:, p, n * 64 + 63:n * 64 + 64], ALU.mult)
            P.tt(f2(tmp), f2(r_), f2(k2), ALU.mult)
            P.tt(rkb[:], tmp[:], _bc(cols[:, 4, :].unsqueeze(2), [128, 4, TB]), ALU.mult)
            pbs = P.bank(P.rr())
            for p in range(4):
                P.mm(pbs[:, p * TB:(p + 1) * TB], blk[:], rkb[:, p, :])
            P.tt(f2(bonus), pbs[:, 0:4 * TB], f2(v_), ALU.mult)
            P.copy(f2(vb), f2(v_), eng="act")

        def rear(tb, part):
            q = tb % 2
            eL, g_, vb, bonus, AR, BK, BKe = eL_[q], g__[q], vb_[q], bonus_[q], AR_[q], BK_[q], BKe_[q]
            t0 = tb * TB
            NU = NCH * 8
            for n in range(NCH if part == 0 else 0):
                ns = slice(n * 64, (n + 1) * 64)
                b2 = P.rr(2)
                ptv = P.bankb(b2)
                ptk = P.bankb(b2 + 1)
                for p in range(4):
                    P.tr(ptv[0:64, p * 128:(p + 1) * 128], vb[:, p, ns], ident[:])
                for p in range(4):
                    P.tr(ptk[0:64, p * 128:(p + 1) * 128], BKe[:, p, n, 0, :], ident[:])
                    P.tr(ptk[0:64, 512 + p * 128:512 + (p + 1) * 128], BKe[:, p, n, 1, :], ident[:])
                P.copy(Vtok[n][:], ptv[0:64, 0:512], eng="act")
                P.copy(Btok[n][:], ptk[0:64, :])
                b4 = P.rr(4)
                pA = P.bank(b4, 4)
                for h in range(8):
                    p, j = h // 2, h % 2
                    js = slice(64 * j, 64 * j + 64)
                    o = (j * 4 + p) * 256
                    rhs = AR[js, p, n, :, :].rearrange("p a b -> p (a b)")
                    P.mm(pA[0:64, o:o + 128], BK[js, p, n, 0, :], rhs)
                    P.mm(pA[0:64, o + 128:o + 256], BK[js, p, n, 1, :], rhs)
                am = AM[n]
                P.tt(am[:].rearrange("t (p j) a b -> t j p (a b)", j=2),
                     pA[0:64, :].rearrange("t (j p c) -> t j p c", j=2, p=4),
                     wmask1[:].rearrange("t (j p a) b -> t j p (a b)", j=2, p=4, a=2), ALU.mult)
                bN = P.rr(2)
                pN = P.bank(bN, 2)
                for h in range(8):
                    p, j = h // 2, h % 2
                    js = slice(64 * j, 64 * j + 64)
                    o = j * 512 + p * 64
                    P.mm(pN[0:64, o:o + 64], AR[js, p, n, 0, :], BK[js, p, n, 0, :])
                P.tt(Qb[0][:, n * 8:(n + 1) * 8, :].rearrange("t (p j) b -> t j p b", j=2),
                     pN[0:64, :].rearrange("t (j x) -> t j x", j=2)[:, :, 0:256].rearrange("t j (p b) -> t j p b", b=64),
                     wmask2[:].rearrange("t (p j) b -> t j p b", j=2), ALU.mult)
                P.tt(Xb[0][:, n * 8:(n + 1) * 8, :], am[:, :, 0, 0:64], identrep[:], ALU.add)
            def P0u(u):
                return AM[u // 8][:, u % 8, 0, 0:64]
            Qc, Pc, Xc = Qb[0], None, Xb[0]
            fl = lambda t: t[:].rearrange("p h b -> p (h b)")
            for r in range(1, 7 if part == 0 else 1):
                Qn, Pn, Xn = Qb[r % 2], Pb[r % 2], Xb[r % 2] if r >= 2 else Xb[0]
                if r <= 5:
                    pQ = P.bank(P.rr(2), 2)
                    for u in range(NU):
                        Pu = P0u(u) if r == 1 else Pc[:, u, :]
                        P.mm(pQ[0:64, u * 64:(u + 1) * 64], Pu, Qc[:, u, :])
                if r <= 4:
                    pP = P.bank(P.rr(2), 2)
                    for u in range(NU):
                        Pu = P0u(u) if r == 1 else Pc[:, u, :]
                        P.mm(pP[0:64, u * 64:(u + 1) * 64], Qc[:, u, :], Pu)
                if r >= 2:
                    pX = P.bank(P.rr(2), 2)
                    for u in range(NU):
                        P.mm(pX[0:64, u * 64:(u + 1) * 64], Qc[:, u, :], Xc[:, u, :])
                if r <= 5:
                    P.copy(fl(Qn), pQ[0:64, 0:NU * 64], eng="act")
                if r <= 4:
                    P.copy(fl(Pn), pP[0:64, 0:NU * 64])
                if r >= 2:
                    P.tt(fl(Xn), pX[0:64, 0:NU * 64], fl(Xc), ALU.add)
                    Xc = Xn
                if r <= 5:
                    Qc = Qn
                if r <= 4:
                    Pc = Pn
            if part == 0:
                return
            XTf = Xb[0]
            for n in range(NCH):
                ns = slice(n * 64, (n + 1) * 64)
                am = AM[n]
                cc = n
                tcnt = tb * NCH + n
                pG2 = P.bank(P.rr())
                for h in range(8):
                    P.mm(pG2[0:64, h * 64:(h + 1) * 64], am[:, h, 1, 0:64], Vtok[cc][:, h * 64:(h + 1) * 64])
                P.copy(G2[:], pG2[0:64, :], eng="act")
                pY2 = P.bank(P.rr())
                for h in range(8):
                    P.mm(pY2[0:64, h * 64:(h + 1) * 64], am[:, h, 1, 64:128], Vtok[cc][:, h * 64:(h + 1) * 64])
                P.copy(Y2[:], pY2[0:64, :], eng="act")
                Tc, Tn = Tb[tcnt % 2], Tb[(tcnt + 1) % 2]
                pGA = P.bank(P.rr(2), 2)
                for h in range(8):
                    p, j = h // 2, h % 2
                    js = slice(64 * j, 64 * j + 64)
                    o = j * 512 + p * 64
                    P.mm(pGA[0:64, o:o + 64], AR[js, p, n, 0, :], Tc[js, p, :])
                P.tt(Gs[:].rearrange("t (p j v) -> t j p v", j=2, v=64),
                     pGA[0:64, :].rearrange("t (j x) -> t j x", j=2)[:, :, 0:256].rearrange("t j (p v) -> t j p v", v=64),
                     G2[:].rearrange("t (p j v) -> t j p v", j=2, v=64), ALU.add)
                pU = P.bank(P.rr())
                for h in range(8):
                    P.mm(pU[0:64, h * 64:(h + 1) * 64], XTf[:, n * 8 + h, :], Gs[:, h * 64:(h + 1) * 64])
                P.copy(Us[:], pU[0:64, :], eng="act")
                pYA = P.bank(P.rr(2), 2)
                for h in range(8):
                    p, j = h // 2, h % 2
                    js = slice(64 * j, 64 * j + 64)
                    o = j * 512 + p * 64
                    P.mm(pYA[0:64, o:o + 64], AR[js, p, n, 1, :], Tc[js, p, :])
                pYB = P.bank(P.rr())
                for h in range(8):
                    P.mm(pYB[0:64, h * 64:(h + 1) * 64], am[:, h, 0, 64:128], Us[:, h * 64:(h + 1) * 64])
                pTU = P.bank(P.rr())
                for p in range(4):
                    P.mm(pTU[:, p * 128:(p + 1) * 128], Btok[cc][:, p * 128:(p + 1) * 128], Us[:, p * 128:(p + 1) * 128],
                         start=True, stop=False)
                    P.mm(pTU[:, p * 128:(p + 1) * 128], Btok[cc][:, 512 + p * 128:512 + (p + 1) * 128],
                         Vtok[cc][:, p * 128:(p + 1) * 128], start=False, stop=True)
                for j in range(2):
                    js = slice(64 * j, 64 * j + 64)
                    P.tt(T[js, :, :], T[js, :, :], _bc(eL[js, :, n * 64 + 63:n * 64 + 64], [64, 4, 64]), ALU.mult)
                    tu = pTU[js, :].rearrange("p (a b) -> p a b", b=128)[:, :, 64 * j:64 * j + 64]
                    P.tt(T[js, :, :], T[js, :, :], tu, ALU.add)
                P.copy(Tn[:], T[:], eng="act")
                P.tt(Yt[:].rearrange("t (p j v) -> t j p v", j=2, v=64),
                     pYA[0:64, :].rearrange("t (j x) -> t j x", j=2)[:, :, 0:256].rearrange("t j (p v) -> t j p v", v=64),
                     Y2[:].rearrange("t (p j v) -> t j p v", j=2, v=64), ALU.add)
                P.tt(Ytb[:], pYB[0:64, :], Yt[:], ALU.add)
                pyt = P.bankb(P.rr())
                for p in range(4):
                    P.tr(pyt[:, p * 64:(p + 1) * 64], Ytb[:, p * 128:(p + 1) * 128], ident[0:64, 0:64])
                P.copy(YT[:, :, ns], pyt[:, 0:256].rearrange("p (a b) -> p a b", b=64), eng="act")
            P.copy(f2(YTb), f2(YT), eng="act")
            pm = P.bank(P.rr())
            for p in range(4):
                P.mm(pm[:, p * TB:(p + 1) * TB], blk[:], YTb[:, p, :])
            P.stt(f2(cen), pm[:, 0:4 * TB], -1.0 / 64.0, f2(YT), ALU.mult, ALU.add)
            P.act(f2(sqp), f2(cen), AF.Square)
            pv2 = P.bank(P.rr())
            for p in range(4):
                P.mm(pv2[:, p * TB:(p + 1) * TB], blk[:], sqp[:, p, :])
            P.act(f2(rnp), pv2[:, 0:4 * TB], AF.Ln, bias=epsg[:, 0:1], scale=1.0 / 64.0)
            P.act(f2(rnp), f2(rnp), AF.Exp, scale=-0.5)
            P.tt(f2(cen), f2(cen), f2(rnp), ALU.mult)
            P.tt(cen[:], cen[:], _bc(cols[:, 5, :].unsqueeze(2), [128, 4, TB]), ALU.mult)
            P.tt(cen[:], cen[:], _bc(cols[:, 6, :].unsqueeze(2), [128, 4, TB]), ALU.add)
            P.tt(f2(cen), f2(cen), f2(bonus), ALU.add)
            P.tt(self.y_rwkvT[:, :, t0:t0 + TB], cen[:], g_[:], ALU.mult)

        import os
        zg = int(os.environ.get("ZG", "0"))
        zs = int(os.environ.get("ZS", "6"))
        front(0, 0)
        for tb in range(NB):
            front(tb, 1)
            rear(tb, 0)
            if tb + 1 < NB and zg > 0:
                zsave = P.zip_gran
                P.zip_gran = zg
                P.zipped([lambda tb=tb: rear(tb, 1), lambda tb=tb: front(tb + 1, 0)], split=[zs, 8 - zs])
                P.zip_gran = zsave
            else:
                rear(tb, 1)
                if tb + 1 < NB:
                    front(tb + 1, 0)
        P.release(m)

    def phase_ret(self, l):
        P = self.P
        m = P.mark()
        cst = _get_consts()
        w_in = self.prm_("w_in")
        cos = P.alloc("cos", [128, S], BF16)
        sins = P.alloc("sins", [128, S], BF16)
        P.dma("sp", cos[:], self.cst("cos"))
        P.dma("sp", sins[:], self.cst("sins"))
        rmask = P.alloc("rmask", [128, 4, 128], F32)
        P.dma("sp", rmask[:], self.cst("rmask"))
        qdec = P.alloc("qdec", [128, 4, 512], BF16)
        P.dma("sp", qdec[:], self.cst("qdec"))
        kdec = P.alloc("kdec", [128, 4], F32)
        P.dma("sp", kdec[:], self.cst("kdec"))
        pswap = P.alloc("pswap", [128, 128], BF16)
        P.dma("sp", pswap[:], self.cst("pswap"))
        gng = P.alloc("gng", [128, 512], F32)
        P.dma("sp", gng[:], self.prm_("ret_gn_g")[l].partition_broadcast(128))
        self.spill()
        Wh = [P.alloc(f"Wh{i}", [128, 8, 512], BF16) for i in range(2)]
        raw = [P.alloc(f"raw{i}", [128, 512], BF16) for i in range(2)]
        t1 = [P.alloc(f"t1_{i}", [128, 512], F32) for i in range(2)]
        t2 = [P.alloc(f"t2_{i}", [128, 512], F32) for i in range(2)]
        def per2(name, shape, dt):
            return [P.alloc(f"{name}{i}", shape, dt) for i in range(2)]
        qrot_, krot_, qd_ = per2("qrot", [128, S], BF16), per2("krot", [128, S], BF16), per2("qd", [128, S], BF16)
        vtok_, sg_, ktok_ = per2("vtok", [128, NT, 128], BF16), per2("sg", [128, NT, 128], BF16), per2("ktok", [128, NT, 128], BF16)
        state_ = per2("state", [128, 128], F32)
        stateb_ = [per2(f"stateb{i}_", [128, 128], BF16) for i in range(2)]
        scm_ = [per2(f"scm{i}_", [128, 128], BF16) for i in range(2)]
        yn_ = [per2(f"yn{i}_", [128, 128], F32) for i in range(2)]
        yb_ = [per2(f"yb{i}_", [128, 128], BF16) for i in range(2)]
        st1_, mv1_, r1_ = per2("st1", [128, 6], F32), per2("mv1", [128, 2], F32), per2("r1", [128, 2], F32)

        def load_w(h):
            W = Wh[h % 2]
            for j in range(4):
                src = w_in[l, :, j * 512 + h * 128:j * 512 + (h + 1) * 128].rearrange("(c p) n -> p c n", p=128)
                P.dma("pool", W[:, :, j * 128:(j + 1) * 128], src)
            return W

        nh = 1 if self.stage == "ret" and "ret1h" in self.taps else 4

        def front(h):
            W = Wh[h % 2]
            qrot, krot, qd = qrot_[h % 2], krot_[h % 2], qd_[h % 2]
            vtok, sg, ktok = vtok_[h % 2], sg_[h % 2], ktok_[h % 2]
            for j, dst in ((0, qrot), (1, krot)):
                for tg in range(4):
                    tsl = slice(tg * 512, (tg + 1) * 512)
                    k2 = (j * 4 + tg) % 2
                    pa = P.bank(P.rr())
                    for c in range(8):
                        P.mm(pa, W[:, c, j * 128:(j + 1) * 128], self.XT[:, c, tsl], start=(c == 0), stop=(c == 7))
                    P.copy(raw[k2][:], pa, eng="act")
                    pb_ = P.bank(P.rr())
                    P.mm(pb_, pswap[:], raw[k2][:])
                    P.tt(t1[k2][:], pa, cos[:, tsl], ALU.mult)
                    P.tt(t2[k2][:], pb_, sins[:, tsl], ALU.mult)
                    P.tt(dst[:, tsl], t1[k2][:], t2[k2][:], ALU.add)
            for tg in range(4):
                tsl = slice(tg * 512, (tg + 1) * 512)
                P.tt(qd[:, tsl], qrot[:, tsl], qdec[:, h, :], ALU.mult, eng="pool")
            for i in range(NT):
                pv = P.bank(P.rr())[:, 0:256]
                for c in range(8):
                    P.mm(pv, self.XT[:, c, i * 128:(i + 1) * 128], W[:, c, 256:512], start=(c == 0), stop=(c == 7))
                P.copy(vtok[:, i, :], pv[:, 0:128], eng="act")
                P.act(sg[:, i, :], pv[:, 128:256], AF.Silu)
            for g in range(2):
                pt = P.bankb(P.rr())
                for ii in range(8):
                    i = g * 8 + ii
                    P.tr(pt[:, ii * 128:(ii + 1) * 128], krot[:, i * 128:(i + 1) * 128], self.ident[:])
                P.ts(ktok[:, g * 8:(g + 1) * 8, :], pt, kdec[:, h:h + 1], ALU.mult)

        def loop(h):
            q = h % 2
            qrot, krot, qd = qrot_[q], krot_[q], qd_[q]
            vtok, sg, ktok = vtok_[q], sg_[q], ktok_[q]
            state, stateb, scm, yn, yb = state_[q], [stateb_[0][q], stateb_[1][q]], [scm_[0][q], scm_[1][q]], \
                [yn_[0][q], yn_[1][q]], [yb_[0][q], yb_[1][q]]
            st1, mv1, r1 = st1_[q], mv1_[q], r1_[q]
            P.memset(state[:], 0.0)
            P.memset(stateb[0][:], 0.0)
            g128 = float(cst["g128"][h])
            for i in range(NT):
                isl = slice(i * 128, (i + 1) * 128)
                sc, sn = stateb[i % 2], stateb[(i + 1) % 2]
                psc = P.bank(P.rr())[:, 0:128]
                P.mm(psc, krot[:, isl], qrot[:, isl])
                P.tt(scm[i % 2][:], psc, rmask[:, h, :], ALU.mult)
                py = P.bank(P.rr())[:, 0:128]
                P.mm(py, scm[i % 2][:], vtok[:, i, :], start=True, stop=False)
                P.mm(py, qd[:, isl], sc[:], start=False, stop=True)
                if i + 1 < NT:
                    pkv = P.bank(P.rr())[:, 0:128]
                    P.mm(pkv, ktok[:, i, :], vtok[:, i, :])
                    P.stt(state[:], state[:], g128, pkv, ALU.mult, ALU.add)
                    P.copy(sn[:], state[:], eng="act")
                P.op("dve", lambda e, py=py: e.bn_stats(out=st1[:], in_=py), reads=[py], writes=[st1[:]])
                P.op("dve", lambda e: e.bn_aggr(out=mv1[:], in_=st1[:]), reads=[st1[:]], writes=[mv1[:]])
                P.act(r1[:, 0:1], mv1[:, 1:2], AF.Ln, bias=self.eps_t[:, 0:1])
                P.act(r1[:, 1:2], r1[:, 0:1], AF.Exp, scale=-0.5)
                y_ = yn[i % 2]
                P.ts(y_[:], py, mv1[:, 0:1], ALU.subtract, r1[:, 1:2], ALU.mult)
                P.tt(y_[:], y_[:], gng[:, h * 128:(h + 1) * 128], ALU.mult, eng="pool")
                P.tt(yb[i % 2][:], y_[:], sg[:, i, :], ALU.mult, eng="pool")
                pT = P.bankb(P.rr())[:, 0:128]
                P.tr(pT, yb[i % 2][:], self.ident[:])
                P.copy(self.y_retT[:, h, isl], pT, eng="act")

        load_w(0)
        for hp in range((nh + 1) // 2):
            hs = [h for h in (2 * hp, 2 * hp + 1) if h < nh]
            for h in hs:
                if h + 1 < nh:
                    load_w(h + 1)
                    if h + 1 == nh - 1 and self.issue_wr is not None:
                        self.issue_wr()
                        self.issue_wr = None
                front(h)
            P.zipped([lambda h=h: loop(h) for h in hs])
        P.release(m)


def _bc(ap, shape):
    return ap.to_broadcast(list(shape))


def _in_map(inputs, b, kb):
    m = {"x": np.ascontiguousarray(inputs["x"][b])}
    if kb._mem_d is not None:
        m["mem"] = np.ascontiguousarray(inputs["mem"][b])
    for k in kb.prm:
        m[k] = np.ascontiguousarray(inputs[k], dtype=np.float32)
    for k in kb.cst_d:
        m["c_" + k] = _get_consts()[k]
    return m


_NC_CACHE = {}


def kernel(**inputs):
    key = "full"
    if key not in _NC_CACHE:
        kb = K()
        _NC_CACHE[key] = (kb, kb.build())
    kb, nc = _NC_CACHE[key]
    in_maps = [_in_map(inputs, b, kb) for b in range(8)]
    res = run_bass_kernel_spmd(nc, in_maps, core_ids=list(range(8)))
    return np.stack([np.asarray(r["out"], dtype=np.float32) for r in res.results], 0)
```
